# Optimizing a Trainium2 kernel written in Bass

```python
import jax, jax.numpy as jnp
from jax import lax
import numpy as np

D_MODEL = 2048
BATCH = 2
SEQ = 8192
DEPTH = 1

N_META = 16
D_FF = 5632
FFN_RES = 0.5
EPS = 1e-6
DSA_HEADS = 8
DSA_HEAD_DIM = 128
DSA_WIDTH = DSA_HEADS * DSA_HEAD_DIM
IDX_HEADS = 16
IDX_DIM = 64
TOPK_MAX = 256
Q_BLOCK = 128
GLA_HEADS = 4
GLA_DK = 128
GLA_DV = 256
GLA_KEY_WIDTH = GLA_HEADS * GLA_DK
GLA_WIDTH = GLA_HEADS * GLA_DV
GLA_GATE_RANK = 16
GLA_TAU = 16.0
GLA_CHUNK = 64
MIX_WIDTH = DSA_WIDTH + GLA_WIDTH
IN_SPLITS = (DSA_WIDTH, DSA_WIDTH, DSA_WIDTH,
             IDX_HEADS * IDX_DIM, IDX_DIM, IDX_HEADS,
             GLA_KEY_WIDTH, GLA_KEY_WIDTH, GLA_WIDTH,
             GLA_GATE_RANK, GLA_WIDTH)
IN_WIDTH = sum(IN_SPLITS)

kernel_name = "hymba_dsa_gla_macaron_layer"


def rms_norm(x, g):
    x32 = x.astype(jnp.float32)
    y = x32 * lax.rsqrt(jnp.mean(x32 * x32, axis=-1, keepdims=True) + EPS)
    return (y * g.astype(jnp.float32)).astype(x.dtype)


def swiglu(x, w_gate, w_up, w_down):
    return (jax.nn.silu(x @ w_gate) * (x @ w_up)) @ w_down


def dsa_attention(q, k, v, q_idx, k_idx, w_idx, top_k):
    b, t, nh, dh = q.shape
    n_blk = -(-t // Q_BLOCK)
    t_pad = n_blk * Q_BLOCK

    def blocks(a):
        a = jnp.pad(a, [(0, 0), (0, t_pad - t)] + [(0, 0)] * (a.ndim - 2))
        a = a.reshape((b, n_blk, Q_BLOCK) + a.shape[2:])
        return jnp.moveaxis(a, 1, 0)

    pos_q = jnp.arange(t_pad, dtype=jnp.int32).reshape(n_blk, Q_BLOCK)
    key_pos = jnp.arange(t, dtype=jnp.int32)
    scale = DSA_HEAD_DIM ** -0.5
    gather = jax.vmap(lambda a, i: a[i])

    def one_block(args):
        qb, qib, wb, pq = args
        logits = jnp.einsum('bqhd,bsd->bqhs', qib, k_idx)
        score = jnp.einsum('bqhs,bqh->bqs', jax.nn.relu(logits), wb).astype(jnp.float32)
        causal = key_pos[None, :] <= pq[:, None]
        score = jnp.where(causal[None], score, -jnp.inf)
        _, sel = lax.top_k(score, top_k)
        valid = sel <= pq[None, :, None]
        ks = gather(k, sel)
        vs = gather(v, sel)
        s = jnp.einsum('bqhd,bqkhd->bhqk', qb, ks).astype(jnp.float32) * scale
        s = jnp.where(valid[:, None], s, -1e30)
        p = jax.nn.softmax(s, axis=-1).astype(v.dtype)
        return jnp.einsum('bhqk,bqkhd->bqhd', p, vs)

    out = lax.map(one_block, (blocks(q), blocks(q_idx), blocks(w_idx), pos_q))
    out = jnp.moveaxis(out, 0, 1).reshape(b, t_pad, nh, dh)
    return out[:, :t]


def gla_chunked(q, k, v, log_g):
    b, t, h, dk = q.shape
    dv = v.shape[-1]
    lead = GLA_CHUNK - N_META
    f32 = jnp.float32

    def prep(a):
        a = jnp.pad(a, [(0, 0), (lead, 0), (0, 0), (0, 0)])
        n = a.shape[1] // GLA_CHUNK
        a = a.reshape(b, n, GLA_CHUNK, h, a.shape[-1])
        return jnp.moveaxis(a, 1, 0)

    causal = jnp.tril(jnp.ones((GLA_CHUNK, GLA_CHUNK), dtype=bool))[None, :, :, None, None]

    def step(state, inp):
        qi, ki, vi, gi = inp
        qf, kf, vf = qi.astype(f32), ki.astype(f32), vi.astype(f32)
        bcum = jnp.cumsum(gi.astype(f32), axis=1)
        o_inter = jnp.einsum('bchk,bhkv->bchv', qf * jnp.exp(bcum), state)
        diff = bcum[:, :, None] - bcum[:, None, :]
        decay = jnp.exp(jnp.where(causal, diff, -jnp.inf))
        attn = jnp.einsum('bihk,bjhk,bijhk->bhij', qf, kf, decay)
        o_intra = jnp.einsum('bhij,bjhv->bihv', attn, vf)
        b_last = bcum[:, -1]
        k_dec = kf * jnp.exp(b_last[:, None] - bcum)
        state = state * jnp.exp(b_last)[..., None] + jnp.einsum('bchk,bchv->bhkv', k_dec, vf)
        return state, (o_inter + o_intra).astype(v.dtype)

    state0 = jnp.zeros((b, h, dk, dv), f32)
    _, out = lax.scan(step, state0, (prep(q), prep(k), prep(v), prep(log_g)))
    out = jnp.moveaxis(out, 0, 1).reshape(b, lead + t, h, dv)
    return out[:, lead:]


def hybrid_mixer(h, w_in, w_gate_up, b_gate, gla_norm_g, w_out, top_k):
    b, t, _ = h.shape
    proj = h @ w_in
    (dq, dk_, dv_, iq, ik, iw, gq, gk, gv, g_low, g_out) = jnp.split(
        proj, np.cumsum(IN_SPLITS)[:-1].tolist(), axis=-1)
    dq = dq.reshape(b, t, DSA_HEADS, DSA_HEAD_DIM)
    dk_ = dk_.reshape(b, t, DSA_HEADS, DSA_HEAD_DIM)
    dv_ = dv_.reshape(b, t, DSA_HEADS, DSA_HEAD_DIM)
    iq = iq.reshape(b, t, IDX_HEADS, IDX_DIM) * (IDX_DIM ** -0.5)
    iw = iw * (IDX_HEADS ** -0.5)
    o_dsa = dsa_attention(dq, dk_, dv_, iq, ik, iw, top_k).reshape(b, t, DSA_WIDTH)
    gq = gq.reshape(b, t, GLA_HEADS, GLA_DK) * (GLA_DK ** -0.5)
    gk = gk.reshape(b, t, GLA_HEADS, GLA_DK)
    gv = gv.reshape(b, t, GLA_HEADS, GLA_DV)
    gate_logit = (g_low @ w_gate_up + b_gate).astype(jnp.float32)
    log_g = (jax.nn.log_sigmoid(gate_logit) / GLA_TAU).reshape(b, t, GLA_HEADS, GLA_DK)
    o_gla = gla_chunked(gq, gk, gv, log_g)
    o_gla = rms_norm(o_gla, gla_norm_g) * jax.nn.silu(g_out.reshape(b, t, GLA_HEADS, GLA_DV))
    o_gla = o_gla.reshape(b, t, GLA_WIDTH)
    return jnp.concatenate([o_dsa, o_gla], axis=-1) @ w_out


def setup_inputs(seed: int = 0) -> dict:
    key = jax.random.key(seed)
    ks = jax.random.split(key, 20)
    f32 = jnp.float32

    def w(k, shape, fan_in):
        return jax.random.normal(k, shape, f32) * (fan_in ** -0.5)

    def gain(k, shape):
        return 1.0 + 0.02 * jax.random.normal(k, shape, f32)

    L = DEPTH
    return {
        "x": jax.random.normal(ks[0], (BATCH, SEQ, D_MODEL), f32),
        "meta_tokens": jax.random.normal(ks[1], (N_META, D_MODEL), f32),
        "ffn1_pre_g": gain(ks[2], (L, D_MODEL)),
        "ffn1_w_gate": w(ks[3], (L, D_MODEL, D_FF), D_MODEL),
        "ffn1_w_up": w(ks[4], (L, D_MODEL, D_FF), D_MODEL),
        "ffn1_w_down": w(ks[5], (L, D_FF, D_MODEL), D_FF),
        "ffn1_post_g": gain(ks[6], (L, D_MODEL)),
        "mix_pre_g": gain(ks[7], (L, D_MODEL)),
        "w_in": w(ks[8], (L, D_MODEL, IN_WIDTH), D_MODEL),
        "w_gate_up": w(ks[9], (L, GLA_GATE_RANK, GLA_KEY_WIDTH), GLA_GATE_RANK),
        "b_gate": 0.01 * jax.random.normal(ks[10], (L, GLA_KEY_WIDTH), f32),
        "gla_norm_g": gain(ks[11], (L, GLA_DV)),
        "w_out": w(ks[12], (L, MIX_WIDTH, D_MODEL), MIX_WIDTH),
        "mix_post_g": gain(ks[13], (L, D_MODEL)),
        "ffn2_pre_g": gain(ks[14], (L, D_MODEL)),
        "ffn2_w_gate": w(ks[15], (L, D_MODEL, D_FF), D_MODEL),
        "ffn2_w_up": w(ks[16], (L, D_MODEL, D_FF), D_MODEL),
        "ffn2_w_down": w(ks[17], (L, D_FF, D_MODEL), D_FF),
        "ffn2_post_g": gain(ks[18], (L, D_MODEL)),
    }


def reference(x, meta_tokens, ffn1_pre_g, ffn1_w_gate, ffn1_w_up, ffn1_w_down, ffn1_post_g,
              mix_pre_g, w_in, w_gate_up, b_gate, gla_norm_g, w_out, mix_post_g,
              ffn2_pre_g, ffn2_w_gate, ffn2_w_up, ffn2_w_down, ffn2_post_g):
    b = x.shape[0]
    top_k = min(TOPK_MAX, SEQ // 4)
    meta = jnp.broadcast_to(meta_tokens[None].astype(x.dtype), (b, N_META, x.shape[-1]))
    hs = jnp.concatenate([meta, x], axis=1)
    for l in range(DEPTH):
        f = swiglu(rms_norm(hs, ffn1_pre_g[l]), ffn1_w_gate[l], ffn1_w_up[l], ffn1_w_down[l])
        hs = hs + FFN_RES * rms_norm(f, ffn1_post_g[l])
        m = hybrid_mixer(rms_norm(hs, mix_pre_g[l]), w_in[l], w_gate_up[l], b_gate[l],
                         gla_norm_g[l], w_out[l], top_k)
        hs = hs + rms_norm(m, mix_post_g[l])
        f = swiglu(rms_norm(hs, ffn2_pre_g[l]), ffn2_w_gate[l], ffn2_w_up[l], ffn2_w_down[l])
        hs = hs + FFN_RES * rms_norm(f, ffn2_post_g[l])
    return hs[:, N_META:]
```

```python
from contextlib import ExitStack
import numpy as np
import concourse.bass as bass
import concourse.mybir as mybir
from concourse.bass_utils import run_bass_kernel_spmd

F32 = mybir.dt.float32
BF16 = mybir.dt.bfloat16
AF = mybir.ActivationFunctionType
ALU = mybir.AluOpType

NCORES = 8
D = 2048
KC = 16
DFF = 5632
FC = 44
NB = 16
NTOK = NB * 128
NMETA = 16
INW = 7264
EPS = 1e-6
TOPK = 256
N_IT = 16
NEG = -30000.0
GROUPS = [[0, 1, 2, 3], [4, 5, 6, 7]]

C_DQ, C_DK, C_DV, C_IQ, C_IK, C_IW = 0, 1024, 2048, 3072, 4096, 4160
C_GQ, C_GK, C_GV, C_GL, C_GO = 4176, 4688, 5200, 6224, 6240

DEV = dict(skip_ffn1=False, stop=None, dsa_lbs=None, gla_nblk=None, cut=99)


class Prog:
    ENGS = ("pe", "act", "dve", "pool", "sp")
    EPOCH = 16000
    NDMA = {"sp": 20, "pool": 20, "act": 8, "cc": 1}
    uid = [0]
    GLOBAL = dict(ebase={}, esems={}, dsems={}, dbase={}, ncc=0)

    def __init__(self, nc):
        self.nc = nc
        self.ops = []
        self.last_w = {}
        self.readers = {}
        self.since_bar = []
        self.cache = {}
        Prog.uid[0] += 1
        self.tag = f"p{Prog.uid[0]}"

    def add(self, eng, fn, reads=(), writes=(), dma=False, cc=False):
        i = len(self.ops)
        deps = set()
        for r in reads:
            w = self.last_w.get(r)
            if w is not None:
                deps.add(w)
        for w_ in writes:
            w = self.last_w.get(w_)
            if w is not None:
                deps.add(w)
            deps.update(self.readers.get(w_, ()))
        self.ops.append(dict(eng=eng, fn=fn, deps=deps, dma=(dma or cc), cc=cc))
        for r in reads:
            self.readers.setdefault(r, []).append(i)
        for w_ in writes:
            self.last_w[w_] = i
            self.readers[w_] = []
        if dma or cc:
            self.since_bar.append(i)
        return i

    def getpid(self, e):
        if "pid" not in self.cache:
            self.cache["pid"] = e.partition_id()
        return self.cache["pid"]

    def barrier(self):
        last = {}
        for i, o in enumerate(self.ops):
            if o["fn"] is not None and not o["dma"]:
                last[o["eng"]] = i
        deps = set(last.values()) | set(self.since_bar)
        for e in self.ENGS:
            self.ops.append(dict(eng=e, fn=None, deps=set(deps), dma=False, cc=False))
        self.last_w = {}
        self.readers = {}
        self.since_bar = []

    def emit(self):
        nc = self.nc
        ops = self.ops
        ordinal = {}
        cnt = {e: 0 for e in self.ENGS}
        dma_slot = {}
        dcnt = {}
        prev_on_slot = {}
        for i, o in enumerate(ops):
            if o["fn"] is None:
                continue
            if o["dma"]:
                if o["cc"]:
                    key = ("cc", dcnt.get("cc", 0))
                    dcnt["cc"] = dcnt.get("cc", 0) + 1
                else:
                    e = o["eng"]
                    key = (e, dcnt.get(e, 0) % self.NDMA[e])
                    dcnt[e] = dcnt.get(e, 0) + 1
                if key in prev_on_slot:
                    o["deps"].add(prev_on_slot[key])
                prev_on_slot[key] = i
                dma_slot[i] = key
            else:
                ordinal[i] = cnt[o["eng"]]
                cnt[o["eng"]] += 1
        dma_count = {}
        dma_val = {}
        for i, o in enumerate(ops):
            if o["dma"]:
                key = dma_slot[i]
                dma_count[key] = dma_count.get(key, 0) + (1 if o["cc"] else 16)
                dma_val[i] = dma_count[key]
        seen_eng = {e: {e2: -1 for e2 in self.ENGS} for e in self.ENGS}
        seen_dma = {e: {} for e in self.ENGS}
        signaled = set()
        for i, o in enumerate(ops):
            e = o["eng"]
            waits = []
            for d in sorted(o["deps"]):
                od = ops[d]
                if od["fn"] is None:
                    continue
                if od["dma"]:
                    key = dma_slot[d]
                    if seen_dma[e].get(key, 0) >= dma_val[d]:
                        continue
                    seen_dma[e][key] = dma_val[d]
                    waits.append(("dma", d))
                else:
                    e2 = od["eng"]
                    if e2 == e and e == "pe" and not o["dma"]:
                        continue
                    if seen_eng[e][e2] >= ordinal[d]:
                        continue
                    seen_eng[e][e2] = ordinal[d]
                    waits.append(("eng", d))
                    signaled.add(d)
            o["waits"] = waits
        sig_val = {}
        scount = {e: 0 for e in self.ENGS}
        for i, o in enumerate(ops):
            if i in signaled:
                sig_val[i] = scount[o["eng"]]
                scount[o["eng"]] += 1
        G = Prog.GLOBAL
        ebase = {e: G["ebase"].get(e, 0) for e in self.ENGS}
        for i in sig_val:
            sig_val[i] += ebase[ops[i]["eng"]]
        for e in self.ENGS:
            G["ebase"][e] = ebase[e] + scount[e]
            need = G["ebase"][e] // self.EPOCH + 1
            lst = G["esems"].setdefault(e, [])
            while len(lst) < need:
                lst.append(nc.alloc_semaphore(f"s_{e}_{len(lst)}"))
        sems = G["esems"]
        dsems = G["dsems"]
        dbase = {}
        for key in sorted(set(dma_slot.values())):
            if key[0] == "cc":
                key2 = ("cc", G["ncc"])
                G["ncc"] += 1
                dsems[(self.tag, key)] = nc.alloc_semaphore(f"d_cc_{key2[1]}")
                dbase[key] = 0
            else:
                if key not in dsems:
                    dsems[key] = nc.alloc_semaphore(f"d_{key[0]}_{key[1]}")
                dbase[key] = G["dbase"].get(key, 0)
        for i in dma_val:
            dma_val[i] += dbase[dma_slot[i]]
        for key, c in dma_count.items():
            if key[0] != "cc":
                G["dbase"][key] = dbase[key] + c
        tag = self.tag

        def dsem(key):
            return dsems[(tag, key)] if key[0] == "cc" else dsems[key]
        engobj = {"pe": nc.tensor, "act": nc.scalar, "dve": nc.vector, "pool": nc.gpsimd, "sp": nc.sync}

        def run(e):
            eo = engobj[e]
            for i, o in enumerate(ops):
                if o["eng"] != e:
                    continue
                for kind, d in o["waits"]:
                    if kind == "dma":
                        eo.wait_ge(dsem(dma_slot[d]), dma_val[d])
                    else:
                        v = sig_val[d]
                        eo.wait_ge(sems[ops[d]["eng"]][v // self.EPOCH], v % self.EPOCH + 1)
                if o["fn"] is None:
                    continue
                ins = o["fn"](eo)
                if o["cc"]:
                    ins.then_inc(dsem(dma_slot[i]))
                elif o["dma"]:
                    ins.then_inc(dsem(dma_slot[i]), 16)
                elif i in signaled:
                    v = sig_val[i]
                    ins.then_inc(sems[e][v // self.EPOCH], 1)

        with nc.Block() as block:
            @block.tensor
            def _(t):
                run("pe")

            @block.scalar
            def _(t):
                run("act")

            @block.vector
            def _(t):
                run("dve")

            @block.gpsimd
            def _(t):
                run("pool")

            @block.sync
            def _(t):
                run("sp")


class Env:
    def __init__(self, nc):
        self.nc = nc
        self.es = ExitStack()
        self.P = Prog(nc)

    def sb(self, name, shape, dt):
        return self.es.enter_context(self.nc.sbuf_tensor(f"{self.P.tag}_{name}", list(shape), dt))

    def ps(self, name, shape, dt):
        return self.es.enter_context(self.nc.psum_tensor(f"{self.P.tag}_{name}", list(shape), dt))

    def pool(self, name, n, shape, dt, psum=False):
        return RPool(self, name, n, shape, dt, psum)

    def finish(self):
        self.P.barrier()
        self.P.emit()
        self.es.close()


class RPool:
    def __init__(self, env, name, n, shape, dt, psum):
        self.bufs = []
        for k in range(n):
            nm = f"{name}{k}"
            t = env.ps(nm, shape, dt) if psum else env.sb(nm, shape, dt)
            self.bufs.append((t, nm))
        self.i = 0

    def next(self):
        b = self.bufs[self.i % len(self.bufs)]
        self.i += 1
        return b


def make_consts(env, want_f32_ident=False):
    P = env.P
    ones_f = env.sb("ones_f", [128, 128], F32)
    ident_f = env.sb("ident_f", [128, 128], F32)
    ident_bf = env.sb("ident_bf", [128, 128], BF16)
    mhalf = env.sb("mhalf", [128, 1], F32)
    P.add("pool", lambda e: e.memset(ones_f[:], 1.0), writes=["ones_f"])
    P.add("pool", lambda e: e.memset(mhalf[:], -0.5), writes=["mhalf"])
    P.add("pool", lambda e: e.affine_select(out=ident_f[:], in_=ones_f[:], pattern=[[1, 128]],
                                            compare_op=ALU.is_equal, fill=0.0, base=0, channel_multiplier=-1),
          reads=["ones_f"], writes=["ident_f"])
    P.add("dve", lambda e: e.tensor_copy(out=ident_bf[:], in_=ident_f[:]), reads=["ident_f"], writes=["ident_bf"])
    return ones_f, ident_f, ident_bf, mhalf


class FFNEnv:
    def __init__(self, env, gain_aps):
        self.env = env
        P = self.P = env.P
        self.ones_f, self.ident_f, self.ident_bf, self.mhalf = make_consts(env)
        self.stat = env.pool("stat", 24, [128, 1], F32)
        self.ps_T = env.pool("psT", 1, [128, 8, 128], BF16, psum=True)
        self.ps_GU = env.pool("psGU", 3, [128, 512], F32, psum=True)
        self.ps_D = env.pool("psD", 4, [128, 512], F32, psum=True)
        self.xs_pool = env.pool("xs", 2, [128, D], F32)
        self.xnb_pool = env.pool("xnb", 1, [128, D], BF16)
        self.xnT = env.sb("xnT", [128, KC, 512], BF16)
        self.actT = env.sb("actT", [128, FC, 512], BF16)
        self.wbig = env.pool("wbig", 4, [128, KC, 256], BF16)
        self.wd_pool = env.pool("wdp", 2, [128, 4, 512], BF16)
        self.tmp_pool = env.pool("tmpf", 2, [128, 512], F32)
        self.fbuf = env.sb("fbuf", [128, 4, D], F32)
        self.stg_bf = env.pool("stgb", 3, [128, 512], BF16)
        self.stg_f = env.pool("stgf", 3, [128, 512], F32)
        self.gt = {}
        for nm, ap in gain_aps.items():
            t = env.sb("gain_" + nm, [128, D], F32)
            P.add("sp", lambda e, t=t, ap=ap: e.dma_start(out=t[:], in_=ap[:, :]), writes=["gain_" + nm], dma=True)
            self.gt[nm] = (t, "gain_" + nm)
        self.flip = 0

    def rstd_from_ss(self, ss, ss_res, n, width):
        P = self.P
        ms, ms_res = self.stat.next()
        rs, rs_res = self.stat.next()
        mhalf = self.mhalf
        P.add("dve", lambda e: e.tensor_scalar(out=ms[:n, :], in0=ss[:n, :], scalar1=1.0 / width, scalar2=EPS,
                                               op0=ALU.mult, op1=ALU.add), reads=[ss_res], writes=[ms_res])
        P.add("pool", lambda e: e.tensor_tensor(out=rs[:n, :], in0=ms[:n, :], in1=mhalf[:n, :], op=ALU.pow),
              reads=[ms_res, "mhalf"], writes=[rs_res])
        return rs, rs_res

    def evac_copy(self, out_ap, in_ap, reads, writes):
        self.flip += 1
        if self.flip % 2:
            self.P.add("act", lambda e: e.copy(out=out_ap, in_=in_ap), reads=reads, writes=writes)
        else:
            self.P.add("dve", lambda e: e.tensor_copy(out=out_ap, in_=in_ap), reads=reads, writes=writes)

    def transpose_into(self, src_bf, src_res, n, col0):
        P = self.P
        pst, pst_res = self.ps_T.next()
        ident_bf = self.ident_bf
        for half in range(2):
            for k in range(8):
                kc = half * 8 + k
                P.add("pe", lambda e, k=k, kc=kc: e.transpose(out=pst[:, k, :n], in_=src_bf[:n, kc * 128:(kc + 1) * 128],
                                                              identity=ident_bf[:n, :n]),
                      reads=[src_res, "ident_bf"], writes=[pst_res])
            self.evac_copy(self.xnT[:, half * 8:(half + 1) * 8, col0:col0 + n], pst[:, :, :n], [pst_res], ["xnT"])

    def norm_transpose(self, src_ap, src_res, n, gname, col0):
        P = self.P
        g, g_res = self.gt[gname]
        xb, xb_res = self.xnb_pool.next()
        ss, ss_res = self.stat.next()
        P.add("act", lambda e: e.activation(out=xb[:n, :], in_=src_ap, func=AF.Square, accum_out=ss[:n, :]),
              reads=[src_res], writes=[xb_res, ss_res])
        rs, rs_res = self.rstd_from_ss(ss, ss_res, n, D)
        P.add("dve", lambda e: e.scalar_tensor_tensor(out=xb[:n, :], in0=src_ap, scalar=rs[:n, 0:1], in1=g[:n, :],
                                                      op0=ALU.mult, op1=ALU.mult),
              reads=[src_res, rs_res, g_res], writes=[xb_res])
        self.transpose_into(xb, xb_res, n, col0)

    def post_norm_residual(self, fb_ap, fb_res, n, gname, coef, res_ap, res_res, out_ap, out_res):
        P = self.P
        g, g_res = self.gt[gname]
        xb, xb_res = self.xnb_pool.next()
        ss, ss_res = self.stat.next()
        P.add("act", lambda e: e.activation(out=xb[:n, :], in_=fb_ap, func=AF.Square, accum_out=ss[:n, :]),
              reads=[fb_res], writes=[xb_res, ss_res])
        rs, rs_res = self.rstd_from_ss(ss, ss_res, n, D)
        P.add("dve", lambda e: e.scalar_tensor_tensor(out=fb_ap, in0=fb_ap, scalar=rs[:n, 0:1], in1=g[:n, :],
                                                      op0=ALU.mult, op1=ALU.mult),
              reads=[fb_res, rs_res, g_res], writes=[fb_res])
        P.add("dve", lambda e: e.scalar_tensor_tensor(out=out_ap, in0=fb_ap, scalar=float(coef), in1=res_ap,
                                                      op0=ALU.mult, op1=ALU.add),
              reads=[fb_res, res_res], writes=[out_res])

    def ffn_core(self, wg, wu, wd, sbs):
        P = self.P
        xnT, actT, fbuf = self.xnT, self.actT, self.fbuf
        ncols = sum(sbs)
        wpair = None
        for f in range(FC):
            if f % 2 == 0:
                wpair = []
                for w in (wg, wu):
                    wt, wt_res = self.wbig.next()
                    P.add("pool", lambda e, wt=wt, w=w, f=f: e.dma_start(
                        out=wt[:], in_=w[:, f * 128:(f + 2) * 128].rearrange("(kc p) f -> p kc f", p=128)),
                        writes=[wt_res], dma=True)
                    wpair.append((wt, wt_res))
            o = (f % 2) * 128
            banks = []
            for (wt, wt_res) in wpair:
                pb, pb_res = self.ps_GU.next()
                for kc in range(KC):
                    P.add("pe", lambda e, pb=pb, wt=wt, kc=kc, o=o: e.matmul(pb[:, :ncols], lhsT=wt[:, kc, o:o + 128], rhs=xnT[:, kc, :ncols],
                                                                           start=(kc == 0), stop=(kc == KC - 1)),
                          reads=[wt_res, "xnT"], writes=[pb_res])
                banks.append((pb, pb_res))
            (pg, pg_res), (pu, pu_res) = banks
            tm, tm_res = self.tmp_pool.next()
            P.add("act", lambda e, pg=pg, tm=tm: e.activation(out=tm[:, :ncols], in_=pg[:, :ncols], func=AF.Silu),
                  reads=[pg_res], writes=[tm_res])
            P.add("dve", lambda e, pu=pu, tm=tm, f=f: e.tensor_tensor(out=actT[:, f, :ncols], in0=tm[:, :ncols], in1=pu[:, :ncols],
                                                                      op=ALU.mult),
                  reads=[tm_res, pu_res], writes=[f"actT{f}"])
        for dg in range(4):
            dbanks = [self.ps_D.next() for _ in sbs]
            for f4 in range(FC // 4):
                wt, wt_res = self.wd_pool.next()
                P.add("pool", lambda e, wt=wt, f4=f4, dg=dg: e.dma_start(
                    out=wt[:], in_=wd[f4 * 512:(f4 + 1) * 512, dg * 512:(dg + 1) * 512].rearrange("(j p) c -> p j c", p=128)),
                    writes=[wt_res], dma=True)
                for j in range(4):
                    f = f4 * 4 + j
                    c0 = 0
                    for s, n in enumerate(sbs):
                        pb, pb_res = dbanks[s]
                        P.add("pe", lambda e, pb=pb, wt=wt, j=j, f=f, c0=c0, n=n: e.matmul(
                            pb[:n, :], lhsT=actT[:, f, c0:c0 + n], rhs=wt[:, j, :], start=(f == 0), stop=(f == FC - 1)),
                            reads=[wt_res, f"actT{f}"], writes=[pb_res])
                        c0 += n
            for s, n in enumerate(sbs):
                pb, pb_res = dbanks[s]
                self.evac_copy(fbuf[:n, s, dg * 512:(dg + 1) * 512], pb[:n, :], [pb_res], [f"fbuf{s}"])


def build_program():
    nc = bass.Bass("TRN2", target_bir_lowering=False)
    Prog.GLOBAL = dict(ebase={}, esems={}, dsems={}, dbase={}, ncc=0)
    declared = []

    def din(name, shape, dt=F32):
        declared.append(name)
        return nc.dram_tensor(name, list(shape), dt, kind="ExternalInput").ap()

    def dint(name, shape, dt):
        return nc.dram_tensor(name, list(shape), dt).ap()

    def dloc(name, shape, dt):
        return nc.dram_tensor(name, list(shape), dt, addr_space="Local").ap()

    skip1 = DEV["skip_ffn1"]
    stop = DEV["stop"]
    xin = din("xin", [NTOK, D])
    meta_in = din("meta", [NMETA, D])
    wts = {}
    wl = [("win", [D, INW]), ("wgu", [16, 512]), ("bgate", [1, 512])]
    if not skip1:
        wl += [("wg1", [D, DFF]), ("wu1", [D, DFF]), ("wd1", [DFF, D])]
    if stop is None:
        wl += [("wout", [D, D]), ("wg2", [D, DFF]), ("wu2", [D, DFF]), ("wd2", [DFF, D])]
    for nm, shp in wl:
        wts[nm] = din(nm, shp)
    gains = {}
    gl = ["gmpre"] + ([] if skip1 else ["g1pre", "g1post"]) + (["gmpost", "g2pre", "g2post"] if stop is None else [])
    for nm in gl:
        gains[nm] = din(nm, [128, D])
    gla_g_in = din("glag", [128, 256])
    qrel_in = din("qrel", [128, 1])
    kreq_in = din("kreq", [128, NB])
    hsel_in = din("hsel", [128, 4])
    y = nc.dram_tensor("y", [NTOK, D], F32, kind="ExternalOutput").ap()
    dbg = None
    if stop is not None:
        dbg = nc.dram_tensor("dbg", [NTOK, D], F32, kind="ExternalOutput").ap()

    hs1_d = dint("hs1_d", [NTOK, D], F32)
    hs2_d = dint("hs2_d", [NTOK, D], F32)
    QT_d = dint("QT_d", [NB * 128, 1024], BF16)
    iqT_d = dint("iqT_d", [NB * 128, 1024], BF16)
    w_d = dint("w_d", [NTOK, 16], F32)
    sg_d = dint("sg_d", [NTOK, 1024], F32)
    omix_d = dint("omix_d", [NTOK, 1024], F32)
    mine_KT = dint("mine_KT", [NB * 128, 1024], BF16)
    mine_V = dint("mine_V", [NTOK, 1024], BF16)
    mine_ikT = dint("mine_ikT", [NB * 128, 128], BF16)
    mine_G = dint("mine_G", [NTOK, 2048], BF16)
    mine_LG = dint("mine_LG", [NTOK, 512], F32)
    g_KT = dloc("g_KT", [4 * NB * 128, 1024], BF16)
    g_V = dloc("g_V", [4 * NTOK, 1024], BF16)
    g_ikT = dloc("g_ikT", [4 * NB * 128, 128], BF16)
    g_G = dloc("g_G", [4 * NTOK, 2048], BF16)
    g_LG = dloc("g_LG", [4 * NTOK, 512], F32)
    mKT_d = dint("mKT_d", [128, 8 * NMETA], BF16)
    mV_d = dint("mV_d", [NMETA, 1024], BF16)
    mikT_d = dint("mikT_d", [128, NMETA], BF16)
    mG_d = dint("mG_d", [NMETA, 2048], BF16)
    mLG_d = dint("mLG_d", [NMETA, 512], F32)
    mine_OG = dint("mine_OG", [4 * NTOK, 256], F32)
    g_OG = dloc("g_OG", [4 * 4 * NTOK, 256], F32)

    CH = dict(KT=512, V=512, ik=512, G=256, LG=512, OG=1024)

    def gather(P, mine, gath, nrows, R, res):
        for i in range(nrows // R):
            P.add("pool", lambda e, i=i: e.collective_compute(
                "AllGather", ALU.bypass, replica_groups=GROUPS,
                ins=[mine[i * R:(i + 1) * R, :].opt()], outs=[gath[i * 4 * R:(i + 1) * 4 * R, :].opt()]),
                writes=[f"{res}_{i}"], cc=True)
        P.add("pool", lambda e: e.nop(), reads=[f"{res}_{i}" for i in range(nrows // R)], writes=[res])

    def grow(r, lr, R):
        return (lr // R) * 4 * R + r * R + (lr % R)

    env = Env(nc)
    P = env.P
    F = FFNEnv(env, {k: gains[k] for k in gains if k in ("g1pre", "g1post", "gmpre")})
    xnT, fbuf = F.xnT, F.fbuf
    glT = env.sb("glT", [16, 512], F32)
    wgu_sb = env.sb("wgu_sb", [16, 512], F32)
    bg_sb = env.sb("bg_sb", [1, 512], F32)
    P.add("sp", lambda e: e.dma_start(out=wgu_sb[:], in_=wts["wgu"][:, :]), writes=["wgu_sb"], dma=True)
    P.add("sp", lambda e: e.dma_start(out=bg_sb[:], in_=wts["bgate"][:, :]), writes=["bg_sb"], dma=True)

    def fm_chunk(col0, ncols_w, dup=False):
        wt, wt_res = F.wbig.next()
        src = wts["win"][:, col0:col0 + ncols_w].rearrange("(kc p) f -> p kc f", p=128)
        P.add("pool", lambda e: e.dma_start(out=wt[:, :, 0:ncols_w], in_=src), writes=[wt_res], dma=True)
        if dup:
            P.add("pool", lambda e: e.dma_start(out=wt[:, :, ncols_w:2 * ncols_w], in_=src), writes=[wt_res], dma=True)
        return wt, wt_res

    def fm_matmul(wt, wt_res, m, ncols, o=0):
        pb, pb_res = F.ps_GU.next()
        for kc in range(KC):
            P.add("pe", lambda e, kc=kc: e.matmul(pb[:m, :ncols], lhsT=wt[:, kc, o:o + m], rhs=xnT[:, kc, :ncols],
                                                  start=(kc == 0), stop=(kc == KC - 1)),
                  reads=[wt_res, "xnT"], writes=[pb_res])
        return pb, pb_res

    pending = dict(KT=[], V=[], ik=[], G=[], LG=[])
    uniq = [0]

    def phase_a_tile(sbs, src_rows, is_meta, lb0):
        ncols = sum(sbs)
        nsb = len(sbs)
        if not skip1:
            c0 = 0
            for s, n in enumerate(sbs):
                xs, xs_res = F.xs_pool.next()
                P.add("sp", lambda e, xs=xs, s=s, n=n: e.dma_start(out=xs[:n, :], in_=src_rows(s)), writes=[xs_res], dma=True)
                F.norm_transpose(xs[:n, :], xs_res, n, "g1pre", c0)
                c0 += n
            F.ffn_core(wts["wg1"], wts["wu1"], wts["wd1"], sbs)
        c0 = 0
        for s, n in enumerate(sbs):
            if not skip1:
                xs, xs_res = F.xs_pool.next()
                P.add("sp", lambda e, xs=xs, s=s, n=n: e.dma_start(out=xs[:n, :], in_=src_rows(s)), writes=[xs_res], dma=True)
                F.post_norm_residual(fbuf[:n, s, :], f"fbuf{s}", n, "g1post", 0.5, xs[:n, :], xs_res, fbuf[:n, s, :], f"fbuf{s}")
            else:
                P.add("sp", lambda e, s=s, n=n: e.dma_start(out=fbuf[:n, s, :], in_=src_rows(s)), writes=[f"fbuf{s}"], dma=True)
            if not is_meta:
                r0 = (lb0 + s) * 128
                P.add("sp", lambda e, s=s, r0=r0: e.dma_start(out=hs1_d[r0:r0 + 128, :], in_=fbuf[:, s, :]),
                      reads=[f"fbuf{s}"], dma=True)
            F.norm_transpose(fbuf[:n, s, :], f"fbuf{s}", n, "gmpre", c0)
            c0 += n

        def track(key):
            if key is None or is_meta:
                return []
            uniq[0] += 1
            nm = f"out_{key}_{uniq[0]}"
            pending[key].append(nm)
            return [nm]

        def store_fm(pb, pb_res, m, dst_ap, scale=None, key=None):
            st, st_res = F.stg_bf.next()
            if scale is None:
                F.evac_copy(st[:m, :ncols], pb[:m, :ncols], [pb_res], [st_res])
            else:
                P.add("act", lambda e: e.activation(out=st[:m, :ncols], in_=pb[:m, :ncols], func=AF.Copy, scale=float(scale)),
                      reads=[pb_res], writes=[st_res])
            src = st[:m, :ncols] if is_meta else st[:m, :ncols].rearrange("p (l t) -> p l t", t=128)
            P.add("sp", lambda e: e.dma_start(out=dst_ap, in_=src), reads=[st_res], writes=track(key), dma=True)

        def blkview(t2d, h):
            v = t2d.rearrange("(l p) (h t) -> p l h t", p=128, t=128)
            return v[:, lb0:lb0 + nsb, h, :]

        for h in range(8):
            if h % 2 == 0:
                wt, wt_res = fm_chunk(C_DK + h * 128, 256)
            pb, pb_res = fm_matmul(wt, wt_res, 128, ncols, (h % 2) * 128)
            store_fm(pb, pb_res, 128, mKT_d[:, h * NMETA:(h + 1) * NMETA] if is_meta else blkview(mine_KT, h), key="KT")
        wt, wt_res = fm_chunk(C_IK, 64, dup=True)
        pb, pb_res = fm_matmul(wt, wt_res, 128, ncols)
        store_fm(pb, pb_res, 128, mikT_d[:, :] if is_meta else
                 mine_ikT.rearrange("(l p) t -> p l t", p=128)[:, lb0:lb0 + nsb, :], key="ik")
        if not is_meta:
            for h in range(8):
                if h % 2 == 0:
                    wt, wt_res = fm_chunk(C_DQ + h * 128, 256)
                pb, pb_res = fm_matmul(wt, wt_res, 128, ncols, (h % 2) * 128)
                store_fm(pb, pb_res, 128, blkview(QT_d, h))
            for h in range(8):
                if h % 2 == 0:
                    wt, wt_res = fm_chunk(C_IQ + h * 128, 256)
                pb, pb_res = fm_matmul(wt, wt_res, 128, ncols, (h % 2) * 128)
                store_fm(pb, pb_res, 128, blkview(iqT_d, h), scale=64 ** -0.5)
        wt, wt_res = fm_chunk(C_GL, 16)
        pb, pb_res = fm_matmul(wt, wt_res, 16, ncols)
        P.add("act", lambda e, pb=pb: e.copy(out=glT[:16, :ncols], in_=pb[:16, :ncols]), reads=[pb_res], writes=["glT"])

        def tm_group(col0, width, consume):
            wt, wt_res = F.wbig.next()
            P.add("pool", lambda e: e.dma_start(out=wt[:, :, :width],
                                                in_=wts["win"][:, col0:col0 + width].rearrange("(kc p) f -> p kc f", p=128)),
                  writes=[wt_res], dma=True)
            c0 = 0
            for s, n in enumerate(sbs):
                pb, pb_res = F.ps_D.next()
                for kc in range(KC):
                    P.add("pe", lambda e, kc=kc, c0=c0, n=n, pb=pb: e.matmul(pb[:n, :width], lhsT=xnT[:, kc, c0:c0 + n],
                                                                             rhs=wt[:, kc, :width], start=(kc == 0), stop=(kc == KC - 1)),
                          reads=[wt_res, "xnT"], writes=[pb_res])
                consume(s, n, pb, pb_res)
                c0 += n

        def rows(t2d, s, n):
            if is_meta:
                return t2d[0:n, :]
            r0 = (lb0 + s) * 128
            return t2d[r0:r0 + n, :]

        def to_bf(dst_mine, dst_meta, dcol, width, scale=None, key=None):
            def consume(s, n, pb, pb_res):
                st, st_res = F.stg_bf.next()
                if scale is None:
                    F.evac_copy(st[:n, :width], pb[:n, :width], [pb_res], [st_res])
                else:
                    P.add("act", lambda e: e.activation(out=st[:n, :width], in_=pb[:n, :width], func=AF.Copy, scale=float(scale)),
                          reads=[pb_res], writes=[st_res])
                dst = rows(dst_meta if is_meta else dst_mine, s, n)[:, dcol:dcol + width]
                P.add("sp", lambda e: e.dma_start(out=dst, in_=st[:n, :width]), reads=[st_res], writes=track(key), dma=True)
            return consume

        for q in range(4):
            tm_group(C_DV + q * 256, 256, to_bf(mine_V, mV_d, q * 256, 256, key="V"))
        for q in range(2):
            tm_group(C_GQ + q * 256, 256, to_bf(mine_G, mG_d, q * 256, 256, scale=128 ** -0.5, key="G"))
        for q in range(2):
            tm_group(C_GK + q * 256, 256, to_bf(mine_G, mG_d, 512 + q * 256, 256, key="G"))
        for q in range(4):
            tm_group(C_GV + q * 256, 256, to_bf(mine_G, mG_d, 1024 + q * 256, 256, key="G"))
        if not is_meta:
            def cons_iw(s, n, pb, pb_res):
                st, st_res = F.stg_f.next()
                P.add("act", lambda e: e.activation(out=st[:n, :16], in_=pb[:n, :16], func=AF.Copy, scale=0.25),
                      reads=[pb_res], writes=[st_res])
                r0 = (lb0 + s) * 128
                P.add("sp", lambda e: e.dma_start(out=w_d[r0:r0 + n, :], in_=st[:n, :16]), reads=[st_res], dma=True)
            tm_group(C_IW, 16, cons_iw)
            for q in range(4):
                def cons_go(s, n, pb, pb_res, q=q):
                    st, st_res = F.stg_f.next()
                    P.add("act", lambda e: e.activation(out=st[:n, :256], in_=pb[:n, :256], func=AF.Silu),
                          reads=[pb_res], writes=[st_res])
                    r0 = (lb0 + s) * 128
                    P.add("sp", lambda e: e.dma_start(out=sg_d[r0:r0 + n, q * 256:(q + 1) * 256], in_=st[:n, :256]),
                          reads=[st_res], dma=True)
                tm_group(C_GO + q * 256, 256, cons_go)
        c0 = 0
        ones_f = F.ones_f
        for s, n in enumerate(sbs):
            pb, pb_res = F.ps_D.next()
            P.add("pe", lambda e, c0=c0, n=n, pb=pb: e.matmul(pb[:n, :], lhsT=glT[:16, c0:c0 + n], rhs=wgu_sb[:16, :], start=True, stop=False),
                  reads=["glT", "wgu_sb"], writes=[pb_res])
            P.add("pe", lambda e, n=n, pb=pb: e.matmul(pb[:n, :], lhsT=ones_f[0:1, :n], rhs=bg_sb[0:1, :], start=False, stop=True),
                  reads=["ones_f", "bg_sb"], writes=[pb_res])
            t1, t1_res = F.stg_f.next()
            P.add("act", lambda e, n=n, pb=pb, t1=t1: e.activation(out=t1[:n, :], in_=pb[:n, :], func=AF.Exp, scale=-1.0),
                  reads=[pb_res], writes=[t1_res])
            P.add("act", lambda e, n=n, t1=t1: e.activation(out=t1[:n, :], in_=t1[:n, :], func=AF.Ln, bias=1.0),
                  reads=[t1_res], writes=[t1_res])
            P.add("dve", lambda e, n=n, t1=t1: e.tensor_scalar(out=t1[:n, :], in0=t1[:n, :], scalar1=-1.0 / 16.0, scalar2=None, op0=ALU.mult),
                  reads=[t1_res], writes=[t1_res])
            dst = rows(mLG_d if is_meta else mine_LG, s, n)
            P.add("sp", lambda e, n=n, t1=t1, dst=dst: e.dma_start(out=dst, in_=t1[:n, :]), reads=[t1_res], writes=track("LG"), dma=True)
            c0 += n

    GX = dict(KT=(mine_KT, g_KT), V=(mine_V, g_V), ik=(mine_ikT, g_ikT), G=(mine_G, g_G), LG=(mine_LG, g_LG))

    def tile_gathers(t, pend):
        for key, (mine, gath) in GX.items():
            R = CH[key]
            for i in range(512 * t // R, 512 * (t + 1) // R):
                P.add("pool", lambda e, i=i, R=R, mine=mine, gath=gath: e.collective_compute(
                    "AllGather", ALU.bypass, replica_groups=GROUPS,
                    ins=[mine[i * R:(i + 1) * R, :].opt()], outs=[gath[i * 4 * R:(i + 1) * 4 * R, :].opt()]),
                    reads=list(pend[key]), cc=True)

    prev = None
    for t in range(4):
        for k in pending:
            pending[k] = []
        phase_a_tile([128] * 4, lambda s, t=t: xin[(4 * t + s) * 128:(4 * t + s + 1) * 128, :], False, 4 * t)
        if prev is not None:
            tile_gathers(*prev)
        prev = (t, {k: list(v) for k, v in pending.items()})
    tile_gathers(*prev)
    phase_a_tile([NMETA], lambda s: meta_in[:, :], True, 0)
    env.finish()

    if stop == "A":
        return nc, declared
    env = Env(nc)
    P = env.P
    if stop == "AC":
        env.finish()
        return nc, declared
    ones_f, ident_f, ident_bf, mhalf = make_consts(env)
    ident4 = env.sb("ident4", [128, 4, 128], BF16)
    for q in range(4):
        P.add("dve", lambda e, q=q: e.tensor_copy(out=ident4[:, q, :], in_=ident_f[:]), reads=["ident_f"], writes=["ident4"])
    kpos0 = env.sb("kpos0", [128, 512], F32)
    penB = env.sb("penB", [128, 512], F32)
    halfpow = env.sb("halfpow", [128, N_IT], F32)
    qrel = env.sb("qrel", [128, 1], F32)
    kreq = env.sb("kreq", [128, NB], F32)
    P.add("sp", lambda e: e.dma_start(out=qrel[:], in_=qrel_in[:, :]), writes=["qrel"], dma=True)
    P.add("sp", lambda e: e.dma_start(out=kreq[:], in_=kreq_in[:, :]), writes=["kreq"], dma=True)
    P.add("pool", lambda e: e.iota(kpos0[:], pattern=[[1, 512]], base=0, channel_multiplier=0,
                                   allow_small_or_imprecise_dtypes=True), writes=["kpos0"])
    P.add("dve", lambda e: e.tensor_scalar(out=penB[:], in0=kpos0[:], scalar1=qrel[:, 0:1], scalar2=0.5,
                                           op0=ALU.is_le, op1=ALU.subtract), reads=["kpos0", "qrel"], writes=["penB"])
    P.add("dve", lambda e: e.tensor_scalar(out=penB[:], in0=penB[:], scalar1=2e30, scalar2=None, op0=ALU.mult),
          reads=["penB"], writes=["penB"])
    for k in range(N_IT):
        P.add("pool", lambda e, k=k: e.memset(halfpow[:, k:k + 1], 2.0 ** (-k)), writes=["halfpow"])

    SMAX = NMETA + 128 * 64
    score_pool = env.pool("score", 2, [128, SMAX], F32)
    mneg_pool = env.pool("mneg", 2, [128, SMAX], BF16)
    cjunk = env.sb("cjunk", [128, SMAX], BF16)
    Dm = env.sb("Dm", [128, 16, 128], BF16)
    QT = env.sb("QT", [128, 8, 128], BF16)
    iqT = env.sb("iqT", [128, 8, 128], BF16)
    wq = env.sb("wq", [128, 16], F32)
    steps = env.sb("steps", [128, N_IT], F32)
    od = env.sb("od", [128, 1024], F32)
    ik_pool = env.pool("ikp", 3, [128, 512], BF16)
    R_pool = env.pool("Rp", 4, [128, 512], BF16)
    KT_pool = env.pool("KTp", 3, [128, 8, 128], BF16)
    V_pool = env.pool("Vp", 3, [128, 8, 129], BF16)
    PT_pool = env.pool("PTp", 3, [128, 512], BF16)
    st1 = env.pool("st1", 48, [128, 1], F32)
    ps_L = env.pool("psL", 2, [128, 512], F32, psum=True)
    ps_ACC = env.pool("psACC", 1, [128, 512], F32, psum=True)
    ps_ST = env.pool("psST", 2, [128, 512], F32, psum=True)
    ps_O = [env.ps(f"psO{k}", [128, 512], F32) for k in range(3)]
    for (vt, vres) in V_pool.bufs:
        P.add("pool", lambda e, vt=vt: e.memset(vt[:], 1.0), writes=[vres])
    flip = [0]
    if stop == "B1c":
        env.finish()
        return nc, declared

    def obank(h):
        return ps_O[h // 3], f"psO{h // 3}", (h % 3) * 129

    lbs = DEV["dsa_lbs"] if DEV["dsa_lbs"] is not None else list(range(NB))[::-1]
    ctx = {}

    def indexer(lb):
        nkb = 4 * lb + 4
        S = NMETA + 128 * nkb
        score, score_res = score_pool.next()
        mneg, mneg_res = mneg_pool.next()
        ctx[lb] = (score, score_res, mneg, mneg_res)
        P.add("sp", lambda e, lb=lb: e.dma_start(out=iqT[:], in_=iqT_d[lb * 128:(lb + 1) * 128, :].rearrange("p (h t) -> p h t", t=128)),
              writes=["iqT"], dma=True)
        P.add("sp", lambda e, lb=lb: e.dma_start(out=wq[:], in_=w_d[lb * 128:(lb + 1) * 128, :]), writes=["wq"], dma=True)
        for h in range(16):
            P.add("dve", lambda e, h=h: e.tensor_scalar(out=Dm[:, h, :], in0=ident_bf[:], scalar1=wq[:, h:h + 1], scalar2=None, op0=ALU.mult),
                  reads=["ident_bf", "wq"], writes=[f"Dm{h}"])
        for g in range(-1, lb + 1):
            w = NMETA if g < 0 else 512
            col0 = 0 if g < 0 else NMETA + 512 * g
            ikt, ik_res = ik_pool.next()
            if g < 0:
                P.add("sp", lambda e, ikt=ikt: e.dma_start(out=ikt[:, :NMETA], in_=mikT_d[:, :]), writes=[ik_res], dma=True)
            else:
                src = g_ikT.rearrange("(i r l p) t -> p i l r t", i=4, r=4, l=4, p=128)[:, g // 4, g % 4, :, :]
                P.add("sp", lambda e, ikt=ikt, src=src: e.dma_start(out=ikt[:].rearrange("p (r t) -> p r t", t=128), in_=src),
                      reads=["g_ik"], writes=[ik_res], dma=True)
            acc, acc_res = ps_ACC.next()
            pend = None
            for h in range(16):
                L, L_res = ps_L.next()
                pb = (h % 2) * 64
                P.add("pe", lambda e, L=L, h=h, pb=pb, ikt=ikt, w=w: e.matmul(L[:, :w], lhsT=iqT[pb:pb + 64, h // 2, :],
                                                                             rhs=ikt[pb:pb + 64, :w], start=True, stop=True),
                      reads=["iqT", ik_res], writes=[L_res])
                Rt, R_res = R_pool.next()
                P.add("act", lambda e, L=L, Rt=Rt, w=w: e.activation(out=Rt[:, :w], in_=L[:, :w], func=AF.Relu),
                      reads=[L_res], writes=[R_res])
                if pend is not None:
                    pend()
                def pend(h=h, Rt=Rt, R_res=R_res, w=w, acc=acc, acc_res=acc_res):
                    P.add("pe", lambda e: e.matmul(acc[:, :w], lhsT=Dm[:, h, :], rhs=Rt[:, :w], start=(h == 0), stop=(h == 15)),
                          reads=[f"Dm{h}", R_res], writes=[acc_res])
            pend()
            P.add("act", lambda e, acc=acc, col0=col0, w=w, score=score: e.copy(out=score[:, col0:col0 + w], in_=acc[:, :w]),
                  reads=[acc_res], writes=[score_res])
    def bisect(lb):
        nkb = 4 * lb + 4
        S = NMETA + 128 * nkb
        score, score_res, mneg, mneg_res = ctx[lb]
        M, M_res = st1.next()
        lo, lo_res = st1.next()
        thr, thr_res = st1.next()
        cnt, cnt_res = st1.next()
        ge, ge_res = st1.next()
        M2, M2_res = st1.next()
        P.add("dve", lambda e, M=M, S=S, score=score: e.tensor_scalar(out=cjunk[:, :S], in0=score[:, :S], scalar1=1.0, scalar2=None,
                                                         op0=ALU.mult, op1=ALU.max, accum_out=M[:, :]),
              reads=[score_res], writes=["cjunk", M_res])
        P.add("dve", lambda e, M2=M2, S=S, score=score: e.tensor_scalar(out=cjunk[:, :S], in0=score[:, :S], scalar1=-1.0, scalar2=None,
                                                           op0=ALU.mult, op1=ALU.max, accum_out=M2[:, :]),
              reads=[score_res], writes=["cjunk", M2_res])
        P.add("dve", lambda e, M=M, M2=M2: e.tensor_tensor(out=M[:, :], in0=M[:, :], in1=M2[:, :], op=ALU.max),
              reads=[M_res, M2_res], writes=[M_res])
        P.add("dve", lambda e, S=S, score=score: e.tensor_tensor(out=score[:, S - 512:S], in0=score[:, S - 512:S], in1=penB[:], op=ALU.min),
              reads=[score_res, "penB"], writes=[score_res])
        P.add("dve", lambda e, M=M, lo=lo: e.tensor_scalar(out=lo[:, :], in0=M[:, :], scalar1=-1.0, scalar2=None, op0=ALU.mult),
              reads=[M_res], writes=[lo_res])
        P.add("dve", lambda e, M=M: e.tensor_scalar(out=steps[:, :], in0=halfpow[:, :], scalar1=M[:, 0:1], scalar2=None, op0=ALU.mult),
              reads=[M_res, "halfpow"], writes=["steps"])
        for k in range(N_IT):
            P.add("dve", lambda e, k=k, lo=lo, thr=thr: e.tensor_tensor(out=thr[:, :], in0=lo[:, :], in1=steps[:, k:k + 1], op=ALU.add),
                  reads=[lo_res, "steps"], writes=[thr_res])
            P.add("dve", lambda e, S=S, thr=thr, cnt=cnt, score=score: e.tensor_scalar(out=cjunk[:, :S], in0=score[:, :S], scalar1=thr[:, 0:1], scalar2=None,
                                                                          op0=ALU.is_ge, op1=ALU.add, accum_out=cnt[:, :]),
                  reads=[score_res, thr_res], writes=["cjunk", cnt_res])
            P.add("dve", lambda e, lb=lb, cnt=cnt, ge=ge: e.tensor_tensor(out=ge[:, :], in0=cnt[:, :], in1=kreq[:, lb:lb + 1], op=ALU.is_ge),
                  reads=[cnt_res, "kreq"], writes=[ge_res])
            P.add("dve", lambda e, k=k, lo=lo, ge=ge: e.scalar_tensor_tensor(out=lo[:, :], in0=ge[:, :], scalar=steps[:, k:k + 1], in1=lo[:, :],
                                                                             op0=ALU.mult, op1=ALU.add),
                  reads=[ge_res, "steps", lo_res], writes=[lo_res])
        P.add("dve", lambda e, S=S, lo=lo, score=score, mneg=mneg: e.tensor_scalar(out=mneg[:, :S], in0=score[:, :S], scalar1=lo[:, 0:1], scalar2=NEG,
                                                           op0=ALU.is_lt, op1=ALU.mult),
              reads=[score_res, lo_res], writes=[mneg_res])
    def attention(lb):
        nkb = 4 * lb + 4
        score, score_res, mneg, mneg_res = ctx[lb]
        P.add("sp", lambda e, lb=lb: e.dma_start(out=QT[:], in_=QT_d[lb * 128:(lb + 1) * 128, :].rearrange("p (h t) -> p h t", t=128)),
              writes=["QT"], dma=True)
        for kb in range(-1, nkb):
            w = NMETA if kb < 0 else 128
            col0 = 0 if kb < 0 else NMETA + 128 * kb
            kt, kt_res = KT_pool.next()
            vt, vt_res = V_pool.next()
            if kb < 0:
                P.add("sp", lambda e, kt=kt: e.dma_start(out=kt[:, :, :NMETA], in_=mKT_d[:, :].rearrange("p (h t) -> p h t", t=NMETA)),
                      writes=[kt_res], dma=True)
                P.add("sp", lambda e, vt=vt: e.dma_start(out=vt[:NMETA, :, 0:128], in_=mV_d[:, :].rearrange("p (h t) -> p h t", t=128)),
                      writes=[vt_res], dma=True)
            else:
                row0 = grow(kb % 4, (kb // 4) * 128, CH["KT"])
                P.add("sp", lambda e, kt=kt, row0=row0: e.dma_start(out=kt[:], in_=g_KT[row0:row0 + 128, :].rearrange("p (h t) -> p h t", t=128)),
                      reads=["g_KT"], writes=[kt_res], dma=True)
                P.add("sp", lambda e, vt=vt, row0=row0: e.dma_start(out=vt[:, :, 0:128], in_=g_V[row0:row0 + 128, :].rearrange("p (h t) -> p h t", t=128)),
                      reads=["g_V"], writes=[vt_res], dma=True)
            for hg in range(2):
                ST, ST_res = ps_ST.next()
                P.add("pe", lambda e, ST=ST, col0=col0, w=w, mneg=mneg: e.matmul(ST[:w, :], lhsT=mneg[:, col0:col0 + w],
                                                                     rhs=ident4[:].rearrange("p a b -> p (a b)"), start=True, stop=False),
                      reads=[mneg_res, "ident4"], writes=[ST_res])
                for h4 in range(4):
                    h = hg * 4 + h4
                    P.add("pe", lambda e, ST=ST, h=h, h4=h4, kt=kt, w=w: e.matmul(ST[:w, h4 * 128:(h4 + 1) * 128], lhsT=kt[:, h, :w],
                                                                                 rhs=QT[:, h, :], start=False, stop=(h4 == 3)),
                          reads=[kt_res, "QT"], writes=[ST_res])
                PT, PT_res = PT_pool.next()
                P.add("act", lambda e, ST=ST, PT=PT, w=w: e.activation(out=PT[:w, :], in_=ST[:w, :], func=AF.Exp, scale=128 ** -0.5),
                      reads=[ST_res], writes=[PT_res])
                for h4 in range(4):
                    h = hg * 4 + h4
                    ob, ob_res, off = obank(h)
                    P.add("pe", lambda e, ob=ob, off=off, PT=PT, h=h, h4=h4, vt=vt, w=w, kb=kb, nkb=nkb: e.matmul(
                        ob[:, off:off + 129], lhsT=PT[:w, h4 * 128:(h4 + 1) * 128], rhs=vt[:w, h, :],
                        start=(kb < 0 and h % 3 == 0), stop=(kb == nkb - 1), skip_group_check=True),
                        reads=[PT_res, vt_res], writes=[ob_res])
        for h in range(8):
            ob, ob_res, off = obank(h)
            rd, rd_res = st1.next()
            P.add("dve", lambda e, ob=ob, off=off, rd=rd: e.reciprocal(out=rd[:, :], in_=ob[:, off + 128:off + 129]),
                  reads=[ob_res], writes=[rd_res])
            P.add("dve", lambda e, ob=ob, off=off, rd=rd, h=h: e.tensor_scalar(out=od[:, h * 128:(h + 1) * 128], in0=ob[:, off:off + 128],
                                                                               scalar1=rd[:, 0:1], scalar2=None, op0=ALU.mult),
                  reads=[ob_res, rd_res], writes=["od"])
        P.add("sp", lambda e, lb=lb: e.dma_start(out=omix_d[lb * 128:(lb + 1) * 128, :], in_=od[:]), reads=["od"], dma=True)
        if stop is not None:
            P.add("sp", lambda e, lb=lb: e.dma_start(out=dbg[lb * 128:(lb + 1) * 128, 0:1024], in_=od[:]), reads=["od"], dma=True)

    if lbs:
        indexer(lbs[0])
    for i, lb in enumerate(lbs):
        if i + 1 < len(lbs):
            indexer(lbs[i + 1])
        bisect(lb)
        attention(lb)
    env.finish()

    env = Env(nc)
    P = env.P
    ones_f, ident_f, ident_bf, mhalf = make_consts(env)
    TRI = env.sb("TRI", [128, 128], F32)
    BLK = env.sb("BLK", [128, 128], F32)
    SEL = env.sb("SEL", [128, 2], F32)
    P.add("pool", lambda e: e.affine_select(out=TRI[:], in_=ones_f[:], pattern=[[1, 128]], compare_op=ALU.is_ge, fill=0.0,
                                            base=0, channel_multiplier=-1), reads=["ones_f"], writes=["TRI"])
    P.add("pool", lambda e: e.memset(TRI[0:64, 64:128], 0.0), writes=["TRI"])
    P.add("pool", lambda e: e.memset(BLK[:], 0.0), writes=["BLK"])
    P.add("pool", lambda e: e.memset(BLK[0:64, 0:64], 1.0), writes=["BLK"])
    P.add("pool", lambda e: e.memset(BLK[64:128, 64:128], 1.0), writes=["BLK"])
    P.add("pool", lambda e: e.memset(SEL[:], 0.0), writes=["SEL"])
    P.add("pool", lambda e: e.memset(SEL[0:64, 0:1], 1.0), writes=["SEL"])
    P.add("pool", lambda e: e.memset(SEL[64:128, 1:2], 1.0), writes=["SEL"])
    glag = env.sb("glag", [128, 256], F32)
    P.add("sp", lambda e: e.dma_start(out=glag[:], in_=gla_g_in[:, :]), writes=["glag"], dma=True)
    Sst = env.sb("Sst", [128, 256], F32)
    P.add("pool", lambda e: e.memset(Sst[:], 0.0), writes=["Sst"])
    Sbf_pool = env.pool("Sbf", 6, [128, 256], BF16)
    q_pool = env.pool("gq", 3, [128, 128], BF16)
    k_pool = env.pool("gk", 3, [128, 128], BF16)
    v_pool = env.pool("gv", 3, [128, 256], BF16)
    lg_pool = env.pool("glg", 3, [128, 128], F32)
    f128 = env.pool("f128", 8, [128, 128], F32)
    b128 = env.pool("b128", 18, [128, 128], BF16)
    qA_pool = env.pool("qA0", 2, [128, 128], BF16)
    qB_pool = env.pool("q0B", 2, [128, 128], BF16)
    dS_pool = env.pool("dSp", 2, [128, 2], F32)
    og_pool = env.pool("ogp", 2, [128, 256], F32)
    junk = env.sb("junkbf", [128, 256], BF16)
    stg = env.pool("gst", 8, [128, 1], F32)
    for (t, r) in qA_pool.bufs + qB_pool.bufs:
        P.add("pool", lambda e, t=t: e.memset(t[:], 0.0), writes=[r])
    ps_bc = env.ps("ps_bc", [128, 512], F32)
    ps_bl = env.ps("ps_bl", [128, 512], F32)
    ps_dS = env.ps("ps_dS", [128, 512], F32)
    ps_att = env.ps("ps_att", [128, 512], F32)
    ps_o = env.ps("ps_o", [128, 512], F32)
    ps_UA = env.ps("ps_UA", [128, 512], F32)
    ps_UB = env.ps("ps_UB", [128, 512], F32)
    ps_tr = env.ps("ps_tr", [128, 2, 128], BF16)
    Sbf, Sbf_res = Sbf_pool.next()
    P.add("pool", lambda e, Sbf=Sbf: e.memset(Sbf[:], 0.0), writes=[Sbf_res])
    cur = {"S": (Sbf, Sbf_res)}
    U_pool = env.pool("Usb", 6, [128, 256], F32)

    hsel = env.sb("hsel", [128, 4], F32)
    P.add("sp", lambda e: e.dma_start(out=hsel[:], in_=hsel_in[:, :]), writes=["hsel"], dma=True)
    qk4_pool = env.pool("qk4", 2, [128, 1024], BF16)
    v4_pool = env.pool("v4", 2, [128, 1024], BF16)
    lg4_pool = env.pool("lg4", 2, [128, 512], F32)
    mqk4 = env.sb("mqk4", [128, 1024], BF16)
    mv4 = env.sb("mv4", [128, 1024], BF16)
    mlg4 = env.sb("mlg4", [128, 512], F32)
    for t, r in ((mqk4, "mqk4"), (mv4, "mv4"), (mlg4, "mlg4")):
        P.add("pool", lambda e, t=t: e.memset(t[:], 0.0), writes=[r])

    def select_head(dst, dst_res, src, src_res, width, col0):
        P.add("dve", lambda e: e.tensor_scalar(out=dst[:], in0=src[:, col0:col0 + width], scalar1=hsel[:, 0:1], scalar2=None, op0=ALU.mult),
              reads=[src_res, "hsel"], writes=[dst_res])
        for hh in range(1, 4):
            P.add("dve", lambda e, hh=hh: e.scalar_tensor_tensor(out=dst[:], in0=src[:, col0 + hh * width:col0 + (hh + 1) * width],
                                                                 scalar=hsel[:, hh:hh + 1], in1=dst[:], op0=ALU.mult, op1=ALU.add),
                  reads=[src_res, "hsel", dst_res], writes=[dst_res])

    if stop == "B2c":
        env.finish()
        return nc, declared
    def og_gather(i):
        R = CH["OG"]
        P.add("pool", lambda e: e.collective_compute(
            "AllGather", ALU.bypass, replica_groups=GROUPS,
            ins=[mine_OG[i * R:(i + 1) * R, :].opt()], outs=[g_OG[i * 4 * R:(i + 1) * 4 * R, :].opt()]),
            reads=[f"og_out_{g}" for g in range(8 * i, 8 * i + 8)], cc=True)

    nblk = DEV["gla_nblk"] if DEV["gla_nblk"] is not None else 64
    def stage1(gb):
        if gb < 0:
            qk4, qk4_res, v4, v4_res, lg4, lg4_res = mqk4, "mqk4", mv4, "mv4", mlg4, "mlg4"
            P.add("sp", lambda e: e.dma_start(out=mqk4[48:64, :], in_=mG_d[:, 0:1024]), writes=["mqk4"], dma=True)
            P.add("sp", lambda e: e.dma_start(out=mv4[48:64, :], in_=mG_d[:, 1024:2048]), writes=["mv4"], dma=True)
            P.add("sp", lambda e: e.dma_start(out=mlg4[48:64, :], in_=mLG_d[:, :]), writes=["mlg4"], dma=True)
        else:
            row0 = grow(gb % 4, (gb // 4) * 128, CH["G"])
            rowl = grow(gb % 4, (gb // 4) * 128, CH["LG"])
            qk4, qk4_res = qk4_pool.next()
            v4, v4_res = v4_pool.next()
            lg4, lg4_res = lg4_pool.next()
            P.add("sp", lambda e, qk4=qk4, row0=row0: e.dma_start(out=qk4[:], in_=g_G[row0:row0 + 128, 0:1024]), reads=["g_G"], writes=[qk4_res], dma=True)
            P.add("sp", lambda e, v4=v4, row0=row0: e.dma_start(out=v4[:], in_=g_G[row0:row0 + 128, 1024:2048]), reads=["g_G"], writes=[v4_res], dma=True)
            P.add("sp", lambda e, lg4=lg4, rowl=rowl: e.dma_start(out=lg4[:], in_=g_LG[rowl:rowl + 128, :]), reads=["g_LG"], writes=[lg4_res], dma=True)
        qt, q_res = q_pool.next()
        kt_, k_res = k_pool.next()
        vt, v_res = v_pool.next()
        lgt, lg_res = lg_pool.next()
        select_head(qt, q_res, qk4, qk4_res, 128, 0)
        select_head(kt_, k_res, qk4, qk4_res, 128, 512)
        select_head(vt, v_res, v4, v4_res, 256, 0)
        select_head(lgt, lg_res, lg4, lg4_res, 128, 0)
        P.add("pe", lambda e, lgt=lgt: e.matmul(ps_bc[:, :128], lhsT=TRI[:], rhs=lgt[:], start=True, stop=True), reads=["TRI", lg_res], writes=["ps_bc"])
        P.add("pe", lambda e, lgt=lgt: e.matmul(ps_bl[:, :128], lhsT=BLK[:], rhs=lgt[:], start=True, stop=True), reads=["BLK", lg_res], writes=["ps_bl"])
        P.add("pe", lambda e, lgt=lgt: e.matmul(ps_dS[:, :128], lhsT=lgt[:], rhs=BLK[:], start=True, stop=True), reads=["BLK", lg_res], writes=["ps_dS"])
        bc, bc_res = f128.next()
        eq, eq_res = f128.next()
        ek, ek_res = f128.next()
        ekd, ekd_res = f128.next()
        dS, dS_res = dS_pool.next()
        P.add("act", lambda e, bc=bc: e.copy(out=bc[:], in_=ps_bc[:, :128]), reads=["ps_bc"], writes=[bc_res])
        P.add("act", lambda e, eq=eq: e.activation(out=eq[:], in_=ps_bc[:, :128], func=AF.Exp), reads=["ps_bc"], writes=[eq_res])
        P.add("act", lambda e, ek=ek: e.activation(out=ek[:], in_=ps_bc[:, :128], func=AF.Exp, scale=-1.0), reads=["ps_bc"], writes=[ek_res])
        P.add("act", lambda e, dS=dS: e.activation(out=dS[:, 0:1], in_=ps_dS[:, 0:1], func=AF.Exp), reads=["ps_dS"], writes=[dS_res])
        P.add("act", lambda e, dS=dS: e.activation(out=dS[:, 1:2], in_=ps_dS[:, 64:65], func=AF.Exp), reads=["ps_dS"], writes=[dS_res])
        P.add("dve", lambda e, bc=bc, ekd=ekd: e.tensor_tensor(out=ekd[:], in0=ps_bl[:, :128], in1=bc[:], op=ALU.subtract),
              reads=["ps_bl", bc_res], writes=[ekd_res])
        P.add("act", lambda e, ekd=ekd: e.activation(out=ekd[:], in_=ekd[:], func=AF.Exp), reads=[ekd_res], writes=[ekd_res])
        qs, qs_res = b128.next()
        ks, ks_res = b128.next()
        kdA, kdA_res = b128.next()
        kdB, kdB_res = b128.next()
        P.add("dve", lambda e, qs=qs, qt=qt, eq=eq: e.tensor_tensor(out=qs[:], in0=qt[:], in1=eq[:], op=ALU.mult), reads=[q_res, eq_res], writes=[qs_res])
        P.add("dve", lambda e, ks=ks, kt_=kt_, ek=ek: e.tensor_tensor(out=ks[:], in0=kt_[:], in1=ek[:], op=ALU.mult), reads=[k_res, ek_res], writes=[ks_res])
        P.add("dve", lambda e, kdA=kdA, kt_=kt_, ekd=ekd: e.scalar_tensor_tensor(out=kdA[:], in0=kt_[:], scalar=SEL[:, 0:1], in1=ekd[:], op0=ALU.mult, op1=ALU.mult),
              reads=[k_res, ekd_res, "SEL"], writes=[kdA_res])
        P.add("dve", lambda e, kdB=kdB, kt_=kt_, ekd=ekd: e.scalar_tensor_tensor(out=kdB[:], in0=kt_[:], scalar=SEL[:, 1:2], in1=ekd[:], op0=ALU.mult, op1=ALU.mult),
              reads=[k_res, ekd_res, "SEL"], writes=[kdB_res])
        P.add("pe", lambda e, qs=qs: e.transpose(out=ps_tr[:, 0, :], in_=qs[:], identity=ident_bf[:]), reads=[qs_res, "ident_bf"], writes=["ps_tr"])
        P.add("pe", lambda e, ks=ks: e.transpose(out=ps_tr[:, 1, :], in_=ks[:], identity=ident_bf[:]), reads=[ks_res, "ident_bf"], writes=["ps_tr"])
        qT, qT_res = b128.next()
        kT, kT_res = b128.next()
        qA, qA_res = qA_pool.next()
        qB, qB_res = qB_pool.next()
        P.add("act", lambda e, qT=qT: e.copy(out=qT[:], in_=ps_tr[:, 0, :]), writes=[qT_res, "ps_tr"])
        P.add("act", lambda e, qA=qA: e.copy(out=qA[:, 0:64], in_=ps_tr[:, 0, 0:64]), writes=[qA_res, "ps_tr"])
        P.add("dve", lambda e, kT=kT: e.tensor_copy(out=kT[:], in_=ps_tr[:, 1, :]), writes=[kT_res, "ps_tr"])
        P.add("dve", lambda e, qB=qB: e.tensor_copy(out=qB[:, 64:128], in_=ps_tr[:, 0, 64:128]), writes=[qB_res, "ps_tr"])
        P.add("pe", lambda e, kT=kT, qT=qT: e.matmul(ps_att[:, :128], lhsT=kT[:], rhs=qT[:], start=True, stop=True), reads=[kT_res, qT_res], writes=["ps_att"])
        am, am_res = b128.next()
        P.add("dve", lambda e, am=am: e.tensor_tensor(out=am[:], in0=ps_att[:, :128], in1=TRI[:], op=ALU.mult), reads=["ps_att", "TRI"], writes=[am_res])
        P.add("pe", lambda e, kdA=kdA, vt=vt: e.matmul(ps_UA[:, :256], lhsT=kdA[:], rhs=vt[:], start=True, stop=True), reads=[kdA_res, v_res], writes=["ps_UA"])
        P.add("pe", lambda e, kdB=kdB, vt=vt: e.matmul(ps_UB[:, :256], lhsT=kdB[:], rhs=vt[:], start=True, stop=True), reads=[kdB_res, v_res], writes=["ps_UB"])
        UA, UA_res = U_pool.next()
        UB, UB_res = U_pool.next()
        P.add("act", lambda e: e.copy(out=UA[:], in_=ps_UA[:, :256]), reads=["ps_UA"], writes=[UA_res])
        P.add("act", lambda e: e.copy(out=UB[:], in_=ps_UB[:, :256]), reads=["ps_UB"], writes=[UB_res])
        return dict(dS=dS, dS_res=dS_res, UA=UA, UA_res=UA_res, UB=UB, UB_res=UB_res, am=am, am_res=am_res, vt=vt, v_res=v_res,
                    qA=qA, qA_res=qA_res, qB=qB, qB_res=qB_res)

    def stage2(gb, d):
        dS, dS_res, UA, UA_res, UB, UB_res = d["dS"], d["dS_res"], d["UA"], d["UA_res"], d["UB"], d["UB_res"]
        am, am_res, vt, v_res, qA, qA_res, qB, qB_res = d["am"], d["am_res"], d["vt"], d["v_res"], d["qA"], d["qA_res"], d["qB"], d["qB_res"]
        SA, SA_res = cur["S"]
        P.add("dve", lambda e: e.scalar_tensor_tensor(out=Sst[:], in0=Sst[:], scalar=dS[:, 0:1], in1=UA[:], op0=ALU.mult, op1=ALU.add),
              reads=["Sst", dS_res, UA_res], writes=["Sst"])
        SB, SB_res = Sbf_pool.next()
        P.add("act", lambda e: e.copy(out=SB[:], in_=Sst[:]), reads=["Sst"], writes=[SB_res])
        P.add("dve", lambda e: e.scalar_tensor_tensor(out=Sst[:], in0=Sst[:], scalar=dS[:, 1:2], in1=UB[:], op0=ALU.mult, op1=ALU.add),
              reads=["Sst", dS_res, UB_res], writes=["Sst"])
        Sn, Sn_res = Sbf_pool.next()
        cur["S"] = (Sn, Sn_res)
        P.add("act", lambda e: e.copy(out=Sn[:], in_=Sst[:]), reads=["Sst"], writes=[Sn_res])
        if gb < 0:
            return
        P.add("pe", lambda e, am=am, vt=vt: e.matmul(ps_o[:, :256], lhsT=am[:], rhs=vt[:], start=True, stop=False), reads=[am_res, v_res], writes=["ps_o"])
        P.add("pe", lambda e, qA=qA, SA=SA: e.matmul(ps_o[:, :256], lhsT=qA[:], rhs=SA[:], start=False, stop=False), reads=[qA_res, SA_res], writes=["ps_o"])
        P.add("pe", lambda e, qB=qB, SB=SB: e.matmul(ps_o[:, :256], lhsT=qB[:], rhs=SB[:], start=False, stop=True), reads=[qB_res, SB_res], writes=["ps_o"])
        ss, ss_res = stg.next()
        ms, ms_res = stg.next()
        rs, rs_res = stg.next()
        P.add("act", lambda e, ss=ss: e.activation(out=junk[:], in_=ps_o[:, :256], func=AF.Square, accum_out=ss[:, :]), reads=["ps_o"], writes=["junk", ss_res])
        P.add("dve", lambda e, ss=ss, ms=ms: e.tensor_scalar(out=ms[:, :], in0=ss[:, :], scalar1=1.0 / 256, scalar2=EPS, op0=ALU.mult, op1=ALU.add),
              reads=[ss_res], writes=[ms_res])
        P.add("pool", lambda e, ms=ms, rs=rs: e.tensor_tensor(out=rs[:, :], in0=ms[:, :], in1=mhalf[:, :], op=ALU.pow), reads=[ms_res, "mhalf"], writes=[rs_res])
        og, og_res = og_pool.next()
        P.add("dve", lambda e, og=og, rs=rs: e.scalar_tensor_tensor(out=og[:], in0=ps_o[:, :256], scalar=rs[:, 0:1], in1=glag[:], op0=ALU.mult, op1=ALU.mult),
              reads=["ps_o", rs_res, "glag"], writes=[og_res])
        P.add("sp", lambda e, og=og, gb=gb: e.dma_start(out=mine_OG[gb * 128:(gb + 1) * 128, :], in_=og[:]), reads=[og_res],
              writes=[f"og_out_{gb}"], dma=True)
        if gb % 8 == 1 and gb >= 9:
            og_gather(gb // 8 - 1)
        if stop is not None:
            P.add("sp", lambda e, og=og, gb=gb: e.dma_start(out=dbg[gb * 128:(gb + 1) * 128, 1024:1280], in_=og[:]), reads=[og_res], dma=True)
    pend_d = stage1(-1)
    for gb in range(-1, nblk):
        nxt = stage1(gb + 1) if gb + 1 < nblk else None
        stage2(gb, pend_d)
        pend_d = nxt
    if nblk == 64:
        og_gather(7)
    env.finish()

    if stop is not None:
        return nc, declared

    env = Env(nc)
    P = env.P
    F = FFNEnv(env, {k: gains[k] for k in ("gmpost", "g2pre", "g2post")})
    xnT, fbuf = F.xnT, F.fbuf
    mixb_pool = F.xnb_pool
    og_view = g_OG.rearrange("(i hh g p) c -> p hh i g c", i=8, hh=4, g=8, p=128)

    def rsel(e):
        if "rsel" not in P.cache:
            P.cache["rsel"] = P.getpid(e) % 4
        return P.cache["rsel"]

    for t in range(4):
        sbs = [128] * 4
        for s in range(4):
            lb = 4 * t + s
            r0 = lb * 128
            od_t, od_res = F.xs_pool.next()
            sg_t, sg_res = F.xs_pool.next()
            P.add("sp", lambda e, od_t=od_t, r0=r0: e.dma_start(out=od_t[:, 0:1024], in_=omix_d[r0:r0 + 128, :]), writes=[od_res], dma=True)
            P.add("pool", lambda e, od_t=od_t, lb=lb: e.dma_start(
                out=od_t[:, 1024:2048].rearrange("p (hh c) -> p hh c", c=256),
                in_=og_view[:, :, lb // 2, 4 * (lb % 2):4 * (lb % 2) + 4, :][:, :, bass.ds(rsel(e), 1), :].rearrange("p hh o c -> p hh (o c)")),
                reads=["g_OG"], writes=[od_res], dma=True)
            P.add("sp", lambda e, sg_t=sg_t, r0=r0: e.dma_start(out=sg_t[:, 0:1024], in_=sg_d[r0:r0 + 128, :]), writes=[sg_res], dma=True)
            mb, mb_res = mixb_pool.next()
            P.add("act", lambda e, mb=mb, od_t=od_t: e.copy(out=mb[:, 0:1024], in_=od_t[:, 0:1024]), reads=[od_res], writes=[mb_res])
            P.add("dve", lambda e, mb=mb, od_t=od_t, sg_t=sg_t: e.tensor_tensor(out=mb[:, 1024:2048], in0=od_t[:, 1024:2048], in1=sg_t[:, 0:1024], op=ALU.mult),
                  reads=[od_res, sg_res], writes=[mb_res])
            F.transpose_into(mb, mb_res, 128, s * 128)
        for cg in range(8):
            wt, wt_res = F.wbig.next()
            P.add("pool", lambda e, wt=wt, cg=cg: e.dma_start(out=wt[:], in_=wts["wout"][:, cg * 256:(cg + 1) * 256].rearrange("(kc p) f -> p kc f", p=128)),
                  writes=[wt_res], dma=True)
            for s in range(4):
                pb, pb_res = F.ps_D.next()
                for kc in range(KC):
                    P.add("pe", lambda e, kc=kc, s=s, pb=pb, wt=wt: e.matmul(pb[:, :256], lhsT=xnT[:, kc, s * 128:(s + 1) * 128], rhs=wt[:, kc, :],
                                                                             start=(kc == 0), stop=(kc == KC - 1)),
                          reads=[wt_res, "xnT"], writes=[pb_res])
                F.evac_copy(fbuf[:, s, cg * 256:(cg + 1) * 256], pb[:, :256], [pb_res], [f"fbuf{s}"])
        for s in range(4):
            r0 = (4 * t + s) * 128
            xs, xs_res = F.xs_pool.next()
            P.add("sp", lambda e, xs=xs, r0=r0: e.dma_start(out=xs[:], in_=hs1_d[r0:r0 + 128, :]), writes=[xs_res], dma=True)
            F.post_norm_residual(fbuf[:, s, :], f"fbuf{s}", 128, "gmpost", 1.0, xs[:], xs_res, fbuf[:, s, :], f"fbuf{s}")
            P.add("sp", lambda e, s=s, r0=r0: e.dma_start(out=hs2_d[r0:r0 + 128, :], in_=fbuf[:, s, :]), reads=[f"fbuf{s}"], writes=[f"hs2_{r0}"], dma=True)
            F.norm_transpose(fbuf[:, s, :], f"fbuf{s}", 128, "g2pre", s * 128)
        F.ffn_core(wts["wg2"], wts["wu2"], wts["wd2"], sbs)
        for s in range(4):
            r0 = (4 * t + s) * 128
            xs, xs_res = F.xs_pool.next()
            P.add("sp", lambda e, xs=xs, r0=r0: e.dma_start(out=xs[:], in_=hs2_d[r0:r0 + 128, :]), reads=[f"hs2_{r0}"], writes=[xs_res], dma=True)
            F.post_norm_residual(fbuf[:, s, :], f"fbuf{s}", 128, "g2post", 0.5, xs[:], xs_res, fbuf[:, s, :], f"fbuf{s}")
            P.add("sp", lambda e, s=s, r0=r0: e.dma_start(out=y[r0:r0 + 128, :], in_=fbuf[:, s, :]), reads=[f"fbuf{s}"], dma=True)
    env.finish()
    return nc, declared


def make_in_maps(inp, declared):
    x = np.asarray(inp["x"], np.float32)
    f = lambda k: np.ascontiguousarray(np.asarray(inp[k], np.float32)[0])
    bc = lambda k, n: np.ascontiguousarray(np.broadcast_to(np.asarray(inp[k], np.float32)[0][None, :], (128, n)))
    names = dict(wg1="ffn1_w_gate", wu1="ffn1_w_up", wd1="ffn1_w_down", win="w_in", wgu="w_gate_up", wout="w_out",
                 wg2="ffn2_w_gate", wu2="ffn2_w_up", wd2="ffn2_w_down")
    gnames = dict(g1pre="ffn1_pre_g", g1post="ffn1_post_g", gmpre="mix_pre_g", gmpost="mix_post_g",
                  g2pre="ffn2_pre_g", g2post="ffn2_post_g")
    shared = {}
    for nm in declared:
        if nm in names:
            shared[nm] = f(names[nm])
        elif nm in gnames:
            shared[nm] = bc(gnames[nm], D)
        elif nm == "bgate":
            shared[nm] = np.asarray(inp["b_gate"], np.float32).reshape(1, 512)
        elif nm == "glag":
            shared[nm] = bc("gla_norm_g", 256)
        elif nm == "meta":
            shared[nm] = np.ascontiguousarray(np.asarray(inp["meta_tokens"], np.float32))
    maps = []
    p = np.arange(128, dtype=np.float32)
    for c in range(NCORES):
        b, r = c // 4, c % 4
        xc = x[b].reshape(64, 128, D)[r::4].reshape(NTOK, D)
        m = dict(shared)
        m["xin"] = np.ascontiguousarray(xc)
        m["qrel"] = (128.0 * r + p).reshape(128, 1).astype(np.float32)
        pos = NMETA + 128.0 * (4 * np.arange(NB)[None, :] + r) + p[:, None]
        m["kreq"] = np.minimum(float(TOPK), pos + 1.0).astype(np.float32)
        hs = np.zeros((128, 4), np.float32)
        hs[:, r] = 1.0
        m["hsel"] = hs
        maps.append(m)
    return maps


_NC_CACHE = {}


def kernel(**inputs):
    if "nc" not in _NC_CACHE:
        _NC_CACHE["nc"] = build_program()
    nc, declared = _NC_CACHE["nc"]
    maps = make_in_maps(inputs, declared)
    res = run_bass_kernel_spmd(nc, maps, core_ids=list(range(NCORES)))
    out = np.zeros((2, 64, 128, D), np.float32)
    for c in range(NCORES):
        b, r = c // 4, c % 4
        out[b, r::4] = np.asarray(res.results[c]["y"], np.float32).reshape(NB, 128, D)
    if DEV["stop"] is not None:
        kernel.dbg = [np.asarray(res.results[c]["dbg"]) for c in range(NCORES)]
    return out.reshape(2, 8192, D)
```

```python
from contextlib import ExitStack
import numpy as np
import concourse.bass as bass
import concourse.mybir as mybir
from concourse.bass_utils import run_bass_kernel_spmd

F32 = mybir.dt.float32
BF16 = mybir.dt.bfloat16
AF = mybir.ActivationFunctionType
ALU = mybir.AluOpType

NCORES = 8
D = 2048
KC = 16
DFF = 5632
FC = 44
NB = 16
NTOK = NB * 128
NMETA = 16
INW = 7264
EPS = 1e-6
TOPK = 256
N_IT = 16
NEG = -30000.0
GROUPS = [[0, 1, 2, 3], [4, 5, 6, 7]]

C_DQ, C_DK, C_DV, C_IQ, C_IK, C_IW = 0, 1024, 2048, 3072, 4096, 4160
C_GQ, C_GK, C_GV, C_GL, C_GO = 4176, 4688, 5200, 6224, 6240

DEV = dict(skip_ffn1=False, stop=None, dsa_lbs=None, gla_nblk=None, cut=99)


class Prog:
    ENGS = ("pe", "act", "dve", "pool", "sp")
    EPOCH = 16000
    NDMA = {"sp": 20, "pool": 20, "act": 8, "cc": 1}
    uid = [0]
    GLOBAL = dict(ebase={}, esems={}, dsems={}, dbase={}, ncc=0)

    def __init__(self, nc):
        self.nc = nc
        self.ops = []
        self.last_w = {}
        self.readers = {}
        self.since_bar = []
        self.cache = {}
        Prog.uid[0] += 1
        self.tag = f"p{Prog.uid[0]}"

    def add(self, eng, fn, reads=(), writes=(), dma=False, cc=False):
        i = len(self.ops)
        deps = set()
        for r in reads:
            w = self.last_w.get(r)
            if w is not None:
                deps.add(w)
        for w_ in writes:
            w = self.last_w.get(w_)
            if w is not None:
                deps.add(w)
            deps.update(self.readers.get(w_, ()))
        self.ops.append(dict(eng=eng, fn=fn, deps=deps, dma=(dma or cc), cc=cc))
        for r in reads:
            self.readers.setdefault(r, []).append(i)
        for w_ in writes:
            self.last_w[w_] = i
            self.readers[w_] = []
        if dma or cc:
            self.since_bar.append(i)
        return i

    def getpid(self, e):
        if "pid" not in self.cache:
            self.cache["pid"] = e.partition_id()
        return self.cache["pid"]

    def barrier(self):
        last = {}
        for i, o in enumerate(self.ops):
            if o["fn"] is not None and not o["dma"]:
                last[o["eng"]] = i
        deps = set(last.values()) | set(self.since_bar)
        for e in self.ENGS:
            self.ops.append(dict(eng=e, fn=None, deps=set(deps), dma=False, cc=False))
        self.last_w = {}
        self.readers = {}
        self.since_bar = []

    def emit(self):
        nc = self.nc
        ops = self.ops
        ordinal = {}
        cnt = {e: 0 for e in self.ENGS}
        dma_slot = {}
        dcnt = {}
        prev_on_slot = {}
        for i, o in enumerate(ops):
            if o["fn"] is None:
                continue
            if o["dma"]:
                if o["cc"]:
                    key = ("cc", dcnt.get("cc", 0))
                    dcnt["cc"] = dcnt.get("cc", 0) + 1
                else:
                    e = o["eng"]
                    key = (e, dcnt.get(e, 0) % self.NDMA[e])
                    dcnt[e] = dcnt.get(e, 0) + 1
                if key in prev_on_slot:
                    o["deps"].add(prev_on_slot[key])
                prev_on_slot[key] = i
                dma_slot[i] = key
            else:
                ordinal[i] = cnt[o["eng"]]
                cnt[o["eng"]] += 1
        dma_count = {}
        dma_val = {}
        for i, o in enumerate(ops):
            if o["dma"]:
                key = dma_slot[i]
                dma_count[key] = dma_count.get(key, 0) + (1 if o["cc"] else 16)
                dma_val[i] = dma_count[key]
        seen_eng = {e: {e2: -1 for e2 in self.ENGS} for e in self.ENGS}
        seen_dma = {e: {} for e in self.ENGS}
        signaled = set()
        for i, o in enumerate(ops):
            e = o["eng"]
            waits = []
            for d in sorted(o["deps"]):
                od = ops[d]
                if od["fn"] is None:
                    continue
                if od["dma"]:
                    key = dma_slot[d]
                    if seen_dma[e].get(key, 0) >= dma_val[d]:
                        continue
                    seen_dma[e][key] = dma_val[d]
                    waits.append(("dma", d))
                else:
                    e2 = od["eng"]
                    if e2 == e and e == "pe" and not o["dma"]:
                        continue
                    if seen_eng[e][e2] >= ordinal[d]:
                        continue
                    seen_eng[e][e2] = ordinal[d]
                    waits.append(("eng", d))
                    signaled.add(d)
            o["waits"] = waits
        sig_val = {}
        scount = {e: 0 for e in self.ENGS}
        for i, o in enumerate(ops):
            if i in signaled:
                sig_val[i] = scount[o["eng"]]
                scount[o["eng"]] += 1
        G = Prog.GLOBAL
        ebase = {e: G["ebase"].get(e, 0) for e in self.ENGS}
        for i in sig_val:
            sig_val[i] += ebase[ops[i]["eng"]]
        for e in self.ENGS:
            G["ebase"][e] = ebase[e] + scount[e]
            need = G["ebase"][e] // self.EPOCH + 1
            lst = G["esems"].setdefault(e, [])
            while len(lst) < need:
                lst.append(nc.alloc_semaphore(f"s_{e}_{len(lst)}"))
        sems = G["esems"]
        dsems = G["dsems"]
        dbase = {}
        for key in sorted(set(dma_slot.values())):
            if key[0] == "cc":
                key2 = ("cc", G["ncc"])
                G["ncc"] += 1
                dsems[(self.tag, key)] = nc.alloc_semaphore(f"d_cc_{key2[1]}")
                dbase[key] = 0
            else:
                if key not in dsems:
                    dsems[key] = nc.alloc_semaphore(f"d_{key[0]}_{key[1]}")
                dbase[key] = G["dbase"].get(key, 0)
        for i in dma_val:
            dma_val[i] += dbase[dma_slot[i]]
        for key, c in dma_count.items():
            if key[0] != "cc":
                G["dbase"][key] = dbase[key] + c
        tag = self.tag

        def dsem(key):
            return dsems[(tag, key)] if key[0] == "cc" else dsems[key]
        engobj = {"pe": nc.tensor, "act": nc.scalar, "dve": nc.vector, "pool": nc.gpsimd, "sp": nc.sync}

        def run(e):
            eo = engobj[e]
            for i, o in enumerate(ops):
                if o["eng"] != e:
                    continue
                for kind, d in o["waits"]:
                    if kind == "dma":
                        eo.wait_ge(dsem(dma_slot[d]), dma_val[d])
                    else:
                        v = sig_val[d]
                        eo.wait_ge(sems[ops[d]["eng"]][v // self.EPOCH], v % self.EPOCH + 1)
                if o["fn"] is None:
                    continue
                ins = o["fn"](eo)
                if o["cc"]:
                    ins.then_inc(dsem(dma_slot[i]))
                elif o["dma"]:
                    ins.then_inc(dsem(dma_slot[i]), 16)
                elif i in signaled:
                    v = sig_val[i]
                    ins.then_inc(sems[e][v // self.EPOCH], 1)

        with nc.Block() as block:
            @block.tensor
            def _(t):
                run("pe")

            @block.scalar
            def _(t):
                run("act")

            @block.vector
            def _(t):
                run("dve")

            @block.gpsimd
            def _(t):
                run("pool")

            @block.sync
            def _(t):
                run("sp")


class Env:
    def __init__(self, nc):
        self.nc = nc
        self.es = ExitStack()
        self.P = Prog(nc)

    def sb(self, name, shape, dt):
        return self.es.enter_context(self.nc.sbuf_tensor(f"{self.P.tag}_{name}", list(shape), dt))

    def ps(self, name, shape, dt):
        return self.es.enter_context(self.nc.psum_tensor(f"{self.P.tag}_{name}", list(shape), dt))

    def pool(self, name, n, shape, dt, psum=False):
        return RPool(self, name, n, shape, dt, psum)

    def finish(self):
        self.P.barrier()
        self.P.emit()
        self.es.close()


class RPool:
    def __init__(self, env, name, n, shape, dt, psum):
        self.bufs = []
        for k in range(n):
            nm = f"{name}{k}"
            t = env.ps(nm, shape, dt) if psum else env.sb(nm, shape, dt)
            self.bufs.append((t, nm))
        self.i = 0

    def next(self):
        b = self.bufs[self.i % len(self.bufs)]
        self.i += 1
        return b


def make_consts(env, want_f32_ident=False):
    P = env.P
    ones_f = env.sb("ones_f", [128, 128], F32)
    ident_f = env.sb("ident_f", [128, 128], F32)
    ident_bf = env.sb("ident_bf", [128, 128], BF16)
    mhalf = env.sb("mhalf", [128, 1], F32)
    P.add("pool", lambda e: e.memset(ones_f[:], 1.0), writes=["ones_f"])
    P.add("pool", lambda e: e.memset(mhalf[:], -0.5), writes=["mhalf"])
    P.add("pool", lambda e: e.affine_select(out=ident_f[:], in_=ones_f[:], pattern=[[1, 128]],
                                            compare_op=ALU.is_equal, fill=0.0, base=0, channel_multiplier=-1),
          reads=["ones_f"], writes=["ident_f"])
    P.add("dve", lambda e: e.tensor_copy(out=ident_bf[:], in_=ident_f[:]), reads=["ident_f"], writes=["ident_bf"])
    return ones_f, ident_f, ident_bf, mhalf


class FFNEnv:
    def __init__(self, env, gain_aps):
        self.env = env
        P = self.P = env.P
        self.ones_f, self.ident_f, self.ident_bf, self.mhalf = make_consts(env)
        self.stat = env.pool("stat", 24, [128, 1], F32)
        self.ps_T = env.pool("psT", 1, [128, 8, 128], BF16, psum=True)
        self.ps_GU = env.pool("psGU", 3, [128, 512], F32, psum=True)
        self.ps_D = env.pool("psD", 4, [128, 512], F32, psum=True)
        self.xs_pool = env.pool("xs", 2, [128, D], F32)
        self.xnb_pool = env.pool("xnb", 1, [128, D], BF16)
        self.xnT = env.sb("xnT", [128, KC, 512], BF16)
        self.actT = env.sb("actT", [128, FC, 512], BF16)
        self.wbig = env.pool("wbig", 4, [128, KC, 256], BF16)
        self.wd_pool = env.pool("wdp", 3, [128, 4, 512], BF16)
        self.tmp_pool = env.pool("tmpf", 2, [128, 512], F32)
        self.fbuf = env.sb("fbuf", [128, 4, D], F32)
        self.stg_bf = env.pool("stgb", 3, [128, 512], BF16)
        self.stg_f = env.pool("stgf", 3, [128, 512], F32)
        self.gt = {}
        for nm, ap in gain_aps.items():
            t = env.sb("gain_" + nm, [128, D], F32)
            P.add("sp", lambda e, t=t, ap=ap: e.dma_start(out=t[:], in_=ap[:, :]), writes=["gain_" + nm], dma=True)
            self.gt[nm] = (t, "gain_" + nm)
        self.flip = 0

    def rstd_from_ss(self, ss, ss_res, n, width):
        P = self.P
        ms, ms_res = self.stat.next()
        rs, rs_res = self.stat.next()
        mhalf = self.mhalf
        P.add("dve", lambda e: e.tensor_scalar(out=ms[:n, :], in0=ss[:n, :], scalar1=1.0 / width, scalar2=EPS,
                                               op0=ALU.mult, op1=ALU.add), reads=[ss_res], writes=[ms_res])
        P.add("pool", lambda e: e.tensor_tensor(out=rs[:n, :], in0=ms[:n, :], in1=mhalf[:n, :], op=ALU.pow),
              reads=[ms_res, "mhalf"], writes=[rs_res])
        return rs, rs_res

    def evac_copy(self, out_ap, in_ap, reads, writes):
        self.flip += 1
        if self.flip % 2:
            self.P.add("act", lambda e: e.copy(out=out_ap, in_=in_ap), reads=reads, writes=writes)
        else:
            self.P.add("dve", lambda e: e.tensor_copy(out=out_ap, in_=in_ap), reads=reads, writes=writes)

    def transpose_into(self, src_bf, src_res, n, col0):
        P = self.P
        pst, pst_res = self.ps_T.next()
        ident_bf = self.ident_bf
        for half in range(2):
            for k in range(8):
                kc = half * 8 + k
                P.add("pe", lambda e, k=k, kc=kc: e.transpose(out=pst[:, k, :n], in_=src_bf[:n, kc * 128:(kc + 1) * 128],
                                                              identity=ident_bf[:n, :n]),
                      reads=[src_res, "ident_bf"], writes=[pst_res])
            self.evac_copy(self.xnT[:, half * 8:(half + 1) * 8, col0:col0 + n], pst[:, :, :n], [pst_res], ["xnT"])

    def norm_transpose(self, src_ap, src_res, n, gname, col0):
        P = self.P
        g, g_res = self.gt[gname]
        xb, xb_res = self.xnb_pool.next()
        ss, ss_res = self.stat.next()
        P.add("act", lambda e: e.activation(out=xb[:n, :], in_=src_ap, func=AF.Square, accum_out=ss[:n, :]),
              reads=[src_res], writes=[xb_res, ss_res])
        rs, rs_res = self.rstd_from_ss(ss, ss_res, n, D)
        P.add("dve", lambda e: e.scalar_tensor_tensor(out=xb[:n, :], in0=src_ap, scalar=rs[:n, 0:1], in1=g[:n, :],
                                                      op0=ALU.mult, op1=ALU.mult),
              reads=[src_res, rs_res, g_res], writes=[xb_res])
        self.transpose_into(xb, xb_res, n, col0)

    def post_norm_residual(self, fb_ap, fb_res, n, gname, coef, res_ap, res_res, out_ap, out_res):
        P = self.P
        g, g_res = self.gt[gname]
        xb, xb_res = self.xnb_pool.next()
        ss, ss_res = self.stat.next()
        P.add("act", lambda e: e.activation(out=xb[:n, :], in_=fb_ap, func=AF.Square, accum_out=ss[:n, :]),
              reads=[fb_res], writes=[xb_res, ss_res])
        rs, rs_res = self.rstd_from_ss(ss, ss_res, n, D)
        P.add("dve", lambda e: e.scalar_tensor_tensor(out=fb_ap, in0=fb_ap, scalar=rs[:n, 0:1], in1=g[:n, :],
                                                      op0=ALU.mult, op1=ALU.mult),
              reads=[fb_res, rs_res, g_res], writes=[fb_res])
        P.add("dve", lambda e: e.scalar_tensor_tensor(out=out_ap, in0=fb_ap, scalar=float(coef), in1=res_ap,
                                                      op0=ALU.mult, op1=ALU.add),
              reads=[fb_res, res_res], writes=[out_res])

    def ffn_core(self, wg, wu, wd, sbs):
        P = self.P
        xnT, actT, fbuf = self.xnT, self.actT, self.fbuf
        ncols = sum(sbs)
        wpair = None
        for f in range(FC):
            if f % 2 == 0:
                wpair = []
                for w in (wg, wu):
                    wt, wt_res = self.wbig.next()
                    P.add("pool", lambda e, wt=wt, w=w, f=f: e.dma_start(
                        out=wt[:], in_=w[:, f * 128:(f + 2) * 128].rearrange("(kc p) f -> p kc f", p=128)),
                        writes=[wt_res], dma=True)
                    wpair.append((wt, wt_res))
            o = (f % 2) * 128
            banks = []
            for (wt, wt_res) in wpair:
                pb, pb_res = self.ps_GU.next()
                for kc in range(KC):
                    P.add("pe", lambda e, pb=pb, wt=wt, kc=kc, o=o: e.matmul(pb[:, :ncols], lhsT=wt[:, kc, o:o + 128], rhs=xnT[:, kc, :ncols],
                                                                           start=(kc == 0), stop=(kc == KC - 1)),
                          reads=[wt_res, "xnT"], writes=[pb_res])
                banks.append((pb, pb_res))
            (pg, pg_res), (pu, pu_res) = banks
            tm, tm_res = self.tmp_pool.next()
            P.add("act", lambda e, pg=pg, tm=tm: e.activation(out=tm[:, :ncols], in_=pg[:, :ncols], func=AF.Silu),
                  reads=[pg_res], writes=[tm_res])
            P.add("dve", lambda e, pu=pu, tm=tm, f=f: e.tensor_tensor(out=actT[:, f, :ncols], in0=tm[:, :ncols], in1=pu[:, :ncols],
                                                                      op=ALU.mult),
                  reads=[tm_res, pu_res], writes=[f"actT{f}"])
        for dg in range(4):
            dbanks = [self.ps_D.next() for _ in sbs]
            for f4 in range(FC // 4):
                wt, wt_res = self.wd_pool.next()
                P.add("pool", lambda e, wt=wt, f4=f4, dg=dg: e.dma_start(
                    out=wt[:], in_=wd[f4 * 512:(f4 + 1) * 512, dg * 512:(dg + 1) * 512].rearrange("(j p) c -> p j c", p=128)),
                    writes=[wt_res], dma=True)
                for j in range(4):
                    f = f4 * 4 + j
                    c0 = 0
                    for s, n in enumerate(sbs):
                        pb, pb_res = dbanks[s]
                        P.add("pe", lambda e, pb=pb, wt=wt, j=j, f=f, c0=c0, n=n: e.matmul(
                            pb[:n, :], lhsT=actT[:, f, c0:c0 + n], rhs=wt[:, j, :], start=(f == 0), stop=(f == FC - 1)),
                            reads=[wt_res, f"actT{f}"], writes=[pb_res])
                        c0 += n
            for s, n in enumerate(sbs):
                pb, pb_res = dbanks[s]
                self.evac_copy(fbuf[:n, s, dg * 512:(dg + 1) * 512], pb[:n, :], [pb_res], [f"fbuf{s}"])


def build_program():
    nc = bass.Bass("TRN2", target_bir_lowering=False)
    Prog.GLOBAL = dict(ebase={}, esems={}, dsems={}, dbase={}, ncc=0)
    declared = []

    def din(name, shape, dt=F32):
        declared.append(name)
        return nc.dram_tensor(name, list(shape), dt, kind="ExternalInput").ap()

    def dint(name, shape, dt):
        return nc.dram_tensor(name, list(shape), dt).ap()

    def dloc(name, shape, dt):
        return nc.dram_tensor(name, list(shape), dt, addr_space="Local").ap()

    skip1 = DEV["skip_ffn1"]
    stop = DEV["stop"]
    xin = din("xin", [NTOK, D])
    meta_in = din("meta", [NMETA, D])
    wts = {}
    wl = [("win", [D, INW]), ("wgu", [16, 512]), ("bgate", [1, 512])]
    if not skip1:
        wl += [("wg1", [D, DFF]), ("wu1", [D, DFF]), ("wd1", [DFF, D])]
    if stop is None:
        wl += [("wout", [D, D]), ("wg2", [D, DFF]), ("wu2", [D, DFF]), ("wd2", [DFF, D])]
    for nm, shp in wl:
        wts[nm] = din(nm, shp)
    gains = {}
    gl = ["gmpre"] + ([] if skip1 else ["g1pre", "g1post"]) + (["gmpost", "g2pre", "g2post"] if stop is None else [])
    for nm in gl:
        gains[nm] = din(nm, [128, D])
    gla_g_in = din("glag", [128, 256])
    qrel_in = din("qrel", [128, 1])
    kreq_in = din("kreq", [128, NB])
    hsel_in = din("hsel", [128, 4])
    y = nc.dram_tensor("y", [NTOK, D], F32, kind="ExternalOutput").ap()
    dbg = None
    if stop is not None:
        dbg = nc.dram_tensor("dbg", [NTOK, D], F32, kind="ExternalOutput").ap()

    hs1_d = dint("hs1_d", [NTOK, D], F32)
    hs2_d = dint("hs2_d", [NTOK, D], F32)
    QT_d = dint("QT_d", [NB * 128, 1024], BF16)
    iqT_d = dint("iqT_d", [NB * 128, 1024], BF16)
    w_d = dint("w_d", [NTOK, 16], F32)
    sg_d = dint("sg_d", [NTOK, 1024], F32)
    omix_d = dint("omix_d", [NTOK, 1024], F32)
    mine_KT = dint("mine_KT", [NB * 128, 1024], BF16)
    mine_V = dint("mine_V", [NTOK, 1024], BF16)
    mine_ikT = dint("mine_ikT", [NB * 128, 128], BF16)
    mine_G = dint("mine_G", [NTOK, 2048], BF16)
    mine_LG = dint("mine_LG", [NTOK, 512], F32)
    g_KT = dloc("g_KT", [4 * NB * 128, 1024], BF16)
    g_V = dloc("g_V", [4 * NTOK, 1024], BF16)
    g_ikT = dloc("g_ikT", [4 * NB * 128, 128], BF16)
    g_G = dloc("g_G", [4 * NTOK, 2048], BF16)
    g_LG = dloc("g_LG", [4 * NTOK, 512], F32)
    mKT_d = dint("mKT_d", [128, 8 * NMETA], BF16)
    mV_d = dint("mV_d", [NMETA, 1024], BF16)
    mikT_d = dint("mikT_d", [128, NMETA], BF16)
    mG_d = dint("mG_d", [NMETA, 2048], BF16)
    mLG_d = dint("mLG_d", [NMETA, 512], F32)
    mine_OG = dint("mine_OG", [4 * NTOK, 256], F32)
    g_OG = dloc("g_OG", [4 * 4 * NTOK, 256], F32)

    CH = dict(KT=512, V=512, ik=512, G=256, LG=512, OG=1024)

    def gather(P, mine, gath, nrows, R, res):
        for i in range(nrows // R):
            P.add("pool", lambda e, i=i: e.collective_compute(
                "AllGather", ALU.bypass, replica_groups=GROUPS,
                ins=[mine[i * R:(i + 1) * R, :].opt()], outs=[gath[i * 4 * R:(i + 1) * 4 * R, :].opt()]),
                writes=[f"{res}_{i}"], cc=True)
        P.add("pool", lambda e: e.nop(), reads=[f"{res}_{i}" for i in range(nrows // R)], writes=[res])

    def grow(r, lr, R):
        return (lr // R) * 4 * R + r * R + (lr % R)

    env = Env(nc)
    P = env.P
    F = FFNEnv(env, {k: gains[k] for k in gains if k in ("g1pre", "g1post", "gmpre")})
    xnT, fbuf = F.xnT, F.fbuf
    glT = env.sb("glT", [16, 512], F32)
    wgu_sb = env.sb("wgu_sb", [16, 512], F32)
    bg_sb = env.sb("bg_sb", [1, 512], F32)
    P.add("sp", lambda e: e.dma_start(out=wgu_sb[:], in_=wts["wgu"][:, :]), writes=["wgu_sb"], dma=True)
    P.add("sp", lambda e: e.dma_start(out=bg_sb[:], in_=wts["bgate"][:, :]), writes=["bg_sb"], dma=True)

    def fm_chunk(col0, ncols_w, dup=False):
        wt, wt_res = F.wbig.next()
        src = wts["win"][:, col0:col0 + ncols_w].rearrange("(kc p) f -> p kc f", p=128)
        P.add("pool", lambda e: e.dma_start(out=wt[:, :, 0:ncols_w], in_=src), writes=[wt_res], dma=True)
        if dup:
            P.add("pool", lambda e: e.dma_start(out=wt[:, :, ncols_w:2 * ncols_w], in_=src), writes=[wt_res], dma=True)
        return wt, wt_res

    def fm_matmul(wt, wt_res, m, ncols, o=0):
        pb, pb_res = F.ps_GU.next()
        for kc in range(KC):
            P.add("pe", lambda e, kc=kc: e.matmul(pb[:m, :ncols], lhsT=wt[:, kc, o:o + m], rhs=xnT[:, kc, :ncols],
                                                  start=(kc == 0), stop=(kc == KC - 1)),
                  reads=[wt_res, "xnT"], writes=[pb_res])
        return pb, pb_res

    pending = dict(KT=[], V=[], ik=[], G=[], LG=[])
    uniq = [0]

    def phase_a_tile(sbs, src_rows, is_meta, lb0):
        ncols = sum(sbs)
        nsb = len(sbs)
        if not skip1:
            c0 = 0
            for s, n in enumerate(sbs):
                xs, xs_res = F.xs_pool.next()
                P.add("sp", lambda e, xs=xs, s=s, n=n: e.dma_start(out=xs[:n, :], in_=src_rows(s)), writes=[xs_res], dma=True)
                F.norm_transpose(xs[:n, :], xs_res, n, "g1pre", c0)
                c0 += n
            F.ffn_core(wts["wg1"], wts["wu1"], wts["wd1"], sbs)
        c0 = 0
        for s, n in enumerate(sbs):
            if not skip1:
                xs, xs_res = F.xs_pool.next()
                P.add("sp", lambda e, xs=xs, s=s, n=n: e.dma_start(out=xs[:n, :], in_=src_rows(s)), writes=[xs_res], dma=True)
                F.post_norm_residual(fbuf[:n, s, :], f"fbuf{s}", n, "g1post", 0.5, xs[:n, :], xs_res, fbuf[:n, s, :], f"fbuf{s}")
            else:
                P.add("sp", lambda e, s=s, n=n: e.dma_start(out=fbuf[:n, s, :], in_=src_rows(s)), writes=[f"fbuf{s}"], dma=True)
            if not is_meta:
                r0 = (lb0 + s) * 128
                P.add("sp", lambda e, s=s, r0=r0: e.dma_start(out=hs1_d[r0:r0 + 128, :], in_=fbuf[:, s, :]),
                      reads=[f"fbuf{s}"], dma=True)
            F.norm_transpose(fbuf[:n, s, :], f"fbuf{s}", n, "gmpre", c0)
            c0 += n

        def track(key):
            if key is None or is_meta:
                return []
            uniq[0] += 1
            nm = f"out_{key}_{uniq[0]}"
            pending[key].append(nm)
            return [nm]

        def store_fm(pb, pb_res, m, dst_ap, scale=None, key=None):
            st, st_res = F.stg_bf.next()
            if scale is None:
                F.evac_copy(st[:m, :ncols], pb[:m, :ncols], [pb_res], [st_res])
            else:
                P.add("act", lambda e: e.activation(out=st[:m, :ncols], in_=pb[:m, :ncols], func=AF.Copy, scale=float(scale)),
                      reads=[pb_res], writes=[st_res])
            src = st[:m, :ncols] if is_meta else st[:m, :ncols].rearrange("p (l t) -> p l t", t=128)
            P.add("sp", lambda e: e.dma_start(out=dst_ap, in_=src), reads=[st_res], writes=track(key), dma=True)

        def blkview(t2d, h):
            v = t2d.rearrange("(l p) (h t) -> p l h t", p=128, t=128)
            return v[:, lb0:lb0 + nsb, h, :]

        for h in range(8):
            if h % 2 == 0:
                wt, wt_res = fm_chunk(C_DK + h * 128, 256)
            pb, pb_res = fm_matmul(wt, wt_res, 128, ncols, (h % 2) * 128)
            store_fm(pb, pb_res, 128, mKT_d[:, h * NMETA:(h + 1) * NMETA] if is_meta else blkview(mine_KT, h), key="KT")
        wt, wt_res = fm_chunk(C_IK, 64, dup=True)
        pb, pb_res = fm_matmul(wt, wt_res, 128, ncols)
        store_fm(pb, pb_res, 128, mikT_d[:, :] if is_meta else
                 mine_ikT.rearrange("(l p) t -> p l t", p=128)[:, lb0:lb0 + nsb, :], key="ik")
        if not is_meta:
            for h in range(8):
                if h % 2 == 0:
                    wt, wt_res = fm_chunk(C_DQ + h * 128, 256)
                pb, pb_res = fm_matmul(wt, wt_res, 128, ncols, (h % 2) * 128)
                store_fm(pb, pb_res, 128, blkview(QT_d, h))
            for h in range(8):
                if h % 2 == 0:
                    wt, wt_res = fm_chunk(C_IQ + h * 128, 256)
                pb, pb_res = fm_matmul(wt, wt_res, 128, ncols, (h % 2) * 128)
                store_fm(pb, pb_res, 128, blkview(iqT_d, h), scale=64 ** -0.5)
        wt, wt_res = fm_chunk(C_GL, 16)
        pb, pb_res = fm_matmul(wt, wt_res, 16, ncols)
        P.add("act", lambda e, pb=pb: e.copy(out=glT[:16, :ncols], in_=pb[:16, :ncols]), reads=[pb_res], writes=["glT"])

        def tm_group(col0, width, consume):
            wt, wt_res = F.wbig.next()
            P.add("pool", lambda e: e.dma_start(out=wt[:, :, :width],
                                                in_=wts["win"][:, col0:col0 + width].rearrange("(kc p) f -> p kc f", p=128)),
                  writes=[wt_res], dma=True)
            c0 = 0
            for s, n in enumerate(sbs):
                pb, pb_res = F.ps_D.next()
                for kc in range(KC):
                    P.add("pe", lambda e, kc=kc, c0=c0, n=n, pb=pb: e.matmul(pb[:n, :width], lhsT=xnT[:, kc, c0:c0 + n],
                                                                             rhs=wt[:, kc, :width], start=(kc == 0), stop=(kc == KC - 1)),
                          reads=[wt_res, "xnT"], writes=[pb_res])
                consume(s, n, pb, pb_res)
                c0 += n

        def rows(t2d, s, n):
            if is_meta:
                return t2d[0:n, :]
            r0 = (lb0 + s) * 128
            return t2d[r0:r0 + n, :]

        def to_bf(dst_mine, dst_meta, dcol, width, scale=None, key=None):
            def consume(s, n, pb, pb_res):
                st, st_res = F.stg_bf.next()
                if scale is None:
                    F.evac_copy(st[:n, :width], pb[:n, :width], [pb_res], [st_res])
                else:
                    P.add("act", lambda e: e.activation(out=st[:n, :width], in_=pb[:n, :width], func=AF.Copy, scale=float(scale)),
                          reads=[pb_res], writes=[st_res])
                dst = rows(dst_meta if is_meta else dst_mine, s, n)[:, dcol:dcol + width]
                P.add("sp", lambda e: e.dma_start(out=dst, in_=st[:n, :width]), reads=[st_res], writes=track(key), dma=True)
            return consume

        for q in range(4):
            tm_group(C_DV + q * 256, 256, to_bf(mine_V, mV_d, q * 256, 256, key="V"))
        for q in range(2):
            tm_group(C_GQ + q * 256, 256, to_bf(mine_G, mG_d, q * 256, 256, scale=128 ** -0.5, key="G"))
        for q in range(2):
            tm_group(C_GK + q * 256, 256, to_bf(mine_G, mG_d, 512 + q * 256, 256, key="G"))
        for q in range(4):
            tm_group(C_GV + q * 256, 256, to_bf(mine_G, mG_d, 1024 + q * 256, 256, key="G"))
        if not is_meta:
            def cons_iw(s, n, pb, pb_res):
                st, st_res = F.stg_f.next()
                P.add("act", lambda e: e.activation(out=st[:n, :16], in_=pb[:n, :16], func=AF.Copy, scale=0.25),
                      reads=[pb_res], writes=[st_res])
                r0 = (lb0 + s) * 128
                P.add("sp", lambda e: e.dma_start(out=w_d[r0:r0 + n, :], in_=st[:n, :16]), reads=[st_res], dma=True)
            tm_group(C_IW, 16, cons_iw)
            for q in range(4):
                def cons_go(s, n, pb, pb_res, q=q):
                    st, st_res = F.stg_f.next()
                    P.add("act", lambda e: e.activation(out=st[:n, :256], in_=pb[:n, :256], func=AF.Silu),
                          reads=[pb_res], writes=[st_res])
                    r0 = (lb0 + s) * 128
                    P.add("sp", lambda e: e.dma_start(out=sg_d[r0:r0 + n, q * 256:(q + 1) * 256], in_=st[:n, :256]),
                          reads=[st_res], dma=True)
                tm_group(C_GO + q * 256, 256, cons_go)
        c0 = 0
        ones_f = F.ones_f
        for s, n in enumerate(sbs):
            pb, pb_res = F.ps_D.next()
            P.add("pe", lambda e, c0=c0, n=n, pb=pb: e.matmul(pb[:n, :], lhsT=glT[:16, c0:c0 + n], rhs=wgu_sb[:16, :], start=True, stop=False),
                  reads=["glT", "wgu_sb"], writes=[pb_res])
            P.add("pe", lambda e, n=n, pb=pb: e.matmul(pb[:n, :], lhsT=ones_f[0:1, :n], rhs=bg_sb[0:1, :], start=False, stop=True),
                  reads=["ones_f", "bg_sb"], writes=[pb_res])
            t1, t1_res = F.stg_f.next()
            P.add("act", lambda e, n=n, pb=pb, t1=t1: e.activation(out=t1[:n, :], in_=pb[:n, :], func=AF.Exp, scale=-1.0),
                  reads=[pb_res], writes=[t1_res])
            P.add("act", lambda e, n=n, t1=t1: e.activation(out=t1[:n, :], in_=t1[:n, :], func=AF.Ln, bias=1.0),
                  reads=[t1_res], writes=[t1_res])
            P.add("dve", lambda e, n=n, t1=t1: e.tensor_scalar(out=t1[:n, :], in0=t1[:n, :], scalar1=-1.0 / 16.0, scalar2=None, op0=ALU.mult),
                  reads=[t1_res], writes=[t1_res])
            dst = rows(mLG_d if is_meta else mine_LG, s, n)
            P.add("sp", lambda e, n=n, t1=t1, dst=dst: e.dma_start(out=dst, in_=t1[:n, :]), reads=[t1_res], writes=track("LG"), dma=True)
            c0 += n

    GX = dict(KT=(mine_KT, g_KT), V=(mine_V, g_V), ik=(mine_ikT, g_ikT), G=(mine_G, g_G), LG=(mine_LG, g_LG))

    def tile_gathers(t, pend):
        for key, (mine, gath) in GX.items():
            R = CH[key]
            for i in range(512 * t // R, 512 * (t + 1) // R):
                P.add("pool", lambda e, i=i, R=R, mine=mine, gath=gath: e.collective_compute(
                    "AllGather", ALU.bypass, replica_groups=GROUPS,
                    ins=[mine[i * R:(i + 1) * R, :].opt()], outs=[gath[i * 4 * R:(i + 1) * 4 * R, :].opt()]),
                    reads=list(pend[key]), cc=True)

    prev = None
    for t in range(4):
        for k in pending:
            pending[k] = []
        phase_a_tile([128] * 4, lambda s, t=t: xin[(4 * t + s) * 128:(4 * t + s + 1) * 128, :], False, 4 * t)
        if prev is not None:
            tile_gathers(*prev)
        prev = (t, {k: list(v) for k, v in pending.items()})
    tile_gathers(*prev)
    phase_a_tile([NMETA], lambda s: meta_in[:, :], True, 0)
    env.finish()

    if stop == "A":
        return nc, declared
    env = Env(nc)
    P = env.P
    if stop == "AC":
        env.finish()
        return nc, declared
    ones_f, ident_f, ident_bf, mhalf = make_consts(env)
    ident4 = env.sb("ident4", [128, 4, 128], BF16)
    for q in range(4):
        P.add("dve", lambda e, q=q: e.tensor_copy(out=ident4[:, q, :], in_=ident_f[:]), reads=["ident_f"], writes=["ident4"])
    kpos0 = env.sb("kpos0", [128, 512], F32)
    penB = env.sb("penB", [128, 512], F32)
    halfpow = env.sb("halfpow", [128, N_IT], F32)
    qrel = env.sb("qrel", [128, 1], F32)
    kreq = env.sb("kreq", [128, NB], F32)
    P.add("sp", lambda e: e.dma_start(out=qrel[:], in_=qrel_in[:, :]), writes=["qrel"], dma=True)
    P.add("sp", lambda e: e.dma_start(out=kreq[:], in_=kreq_in[:, :]), writes=["kreq"], dma=True)
    P.add("pool", lambda e: e.iota(kpos0[:], pattern=[[1, 512]], base=0, channel_multiplier=0,
                                   allow_small_or_imprecise_dtypes=True), writes=["kpos0"])
    P.add("dve", lambda e: e.tensor_scalar(out=penB[:], in0=kpos0[:], scalar1=qrel[:, 0:1], scalar2=0.5,
                                           op0=ALU.is_le, op1=ALU.subtract), reads=["kpos0", "qrel"], writes=["penB"])
    P.add("dve", lambda e: e.tensor_scalar(out=penB[:], in0=penB[:], scalar1=2e30, scalar2=None, op0=ALU.mult),
          reads=["penB"], writes=["penB"])
    for k in range(N_IT):
        P.add("pool", lambda e, k=k: e.memset(halfpow[:, k:k + 1], 2.0 ** (-k)), writes=["halfpow"])

    SMAX = NMETA + 128 * 64
    score_pool = env.pool("score", 2, [128, SMAX], F32)
    mneg_pool = env.pool("mneg", 2, [128, SMAX], BF16)
    cjunk = env.sb("cjunk", [128, SMAX], BF16)
    Dm = env.sb("Dm", [128, 16, 128], BF16)
    QT = env.sb("QT", [128, 8, 128], BF16)
    iqT = env.sb("iqT", [128, 8, 128], BF16)
    wq = env.sb("wq", [128, 16], F32)
    steps = env.sb("steps", [128, N_IT], F32)
    od = env.sb("od", [128, 1024], F32)
    ik_pool = env.pool("ikp", 3, [128, 512], BF16)
    R_pool = env.pool("Rp", 4, [128, 512], BF16)
    KT_pool = env.pool("KTp", 3, [128, 8, 128], BF16)
    V_pool = env.pool("Vp", 3, [128, 8, 129], BF16)
    PT_pool = env.pool("PTp", 3, [128, 512], BF16)
    st1 = env.pool("st1", 48, [128, 1], F32)
    ps_L = env.pool("psL", 2, [128, 512], F32, psum=True)
    ps_ACC = env.pool("psACC", 1, [128, 512], F32, psum=True)
    ps_ST = env.pool("psST", 2, [128, 512], F32, psum=True)
    ps_O = [env.ps(f"psO{k}", [128, 512], F32) for k in range(3)]
    for (vt, vres) in V_pool.bufs:
        P.add("pool", lambda e, vt=vt: e.memset(vt[:], 1.0), writes=[vres])
    flip = [0]
    if stop == "B1c":
        env.finish()
        return nc, declared

    def obank(h):
        return ps_O[h // 3], f"psO{h // 3}", (h % 3) * 129

    lbs = DEV["dsa_lbs"] if DEV["dsa_lbs"] is not None else list(range(NB))[::-1]
    ctx = {}

    def indexer(lb):
        nkb = 4 * lb + 4
        S = NMETA + 128 * nkb
        score, score_res = score_pool.next()
        mneg, mneg_res = mneg_pool.next()
        ctx[lb] = (score, score_res, mneg, mneg_res)
        P.add("sp", lambda e, lb=lb: e.dma_start(out=iqT[:], in_=iqT_d[lb * 128:(lb + 1) * 128, :].rearrange("p (h t) -> p h t", t=128)),
              writes=["iqT"], dma=True)
        P.add("sp", lambda e, lb=lb: e.dma_start(out=wq[:], in_=w_d[lb * 128:(lb + 1) * 128, :]), writes=["wq"], dma=True)
        for h in range(16):
            P.add("dve", lambda e, h=h: e.tensor_scalar(out=Dm[:, h, :], in0=ident_bf[:], scalar1=wq[:, h:h + 1], scalar2=None, op0=ALU.mult),
                  reads=["ident_bf", "wq"], writes=[f"Dm{h}"])
        for g in range(-1, lb + 1):
            w = NMETA if g < 0 else 512
            col0 = 0 if g < 0 else NMETA + 512 * g
            ikt, ik_res = ik_pool.next()
            if g < 0:
                P.add("sp", lambda e, ikt=ikt: e.dma_start(out=ikt[:, :NMETA], in_=mikT_d[:, :]), writes=[ik_res], dma=True)
            else:
                src = g_ikT.rearrange("(i r l p) t -> p i l r t", i=4, r=4, l=4, p=128)[:, g // 4, g % 4, :, :]
                P.add("sp", lambda e, ikt=ikt, src=src: e.dma_start(out=ikt[:].rearrange("p (r t) -> p r t", t=128), in_=src),
                      reads=["g_ik"], writes=[ik_res], dma=True)
            acc, acc_res = ps_ACC.next()
            pend = None
            for h in range(16):
                L, L_res = ps_L.next()
                pb = (h % 2) * 64
                P.add("pe", lambda e, L=L, h=h, pb=pb, ikt=ikt, w=w: e.matmul(L[:, :w], lhsT=iqT[pb:pb + 64, h // 2, :],
                                                                             rhs=ikt[pb:pb + 64, :w], start=True, stop=True),
                      reads=["iqT", ik_res], writes=[L_res])
                Rt, R_res = R_pool.next()
                P.add("act", lambda e, L=L, Rt=Rt, w=w: e.activation(out=Rt[:, :w], in_=L[:, :w], func=AF.Relu),
                      reads=[L_res], writes=[R_res])
                if pend is not None:
                    pend()
                def pend(h=h, Rt=Rt, R_res=R_res, w=w, acc=acc, acc_res=acc_res):
                    P.add("pe", lambda e: e.matmul(acc[:, :w], lhsT=Dm[:, h, :], rhs=Rt[:, :w], start=(h == 0), stop=(h == 15)),
                          reads=[f"Dm{h}", R_res], writes=[acc_res])
            pend()
            P.add("act", lambda e, acc=acc, col0=col0, w=w, score=score: e.copy(out=score[:, col0:col0 + w], in_=acc[:, :w]),
                  reads=[acc_res], writes=[score_res])
    def bisect(lb):
        nkb = 4 * lb + 4
        S = NMETA + 128 * nkb
        score, score_res, mneg, mneg_res = ctx[lb]
        M, M_res = st1.next()
        lo, lo_res = st1.next()
        thr, thr_res = st1.next()
        cnt, cnt_res = st1.next()
        ge, ge_res = st1.next()
        M2, M2_res = st1.next()
        P.add("dve", lambda e, M=M, S=S, score=score: e.tensor_scalar(out=cjunk[:, :S], in0=score[:, :S], scalar1=1.0, scalar2=None,
                                                         op0=ALU.mult, op1=ALU.max, accum_out=M[:, :]),
              reads=[score_res], writes=["cjunk", M_res])
        P.add("dve", lambda e, M2=M2, S=S, score=score: e.tensor_scalar(out=cjunk[:, :S], in0=score[:, :S], scalar1=-1.0, scalar2=None,
                                                           op0=ALU.mult, op1=ALU.max, accum_out=M2[:, :]),
              reads=[score_res], writes=["cjunk", M2_res])
        P.add("dve", lambda e, M=M, M2=M2: e.tensor_tensor(out=M[:, :], in0=M[:, :], in1=M2[:, :], op=ALU.max),
              reads=[M_res, M2_res], writes=[M_res])
        P.add("dve", lambda e, S=S, score=score: e.tensor_tensor(out=score[:, S - 512:S], in0=score[:, S - 512:S], in1=penB[:], op=ALU.min),
              reads=[score_res, "penB"], writes=[score_res])
        P.add("dve", lambda e, M=M, lo=lo: e.tensor_scalar(out=lo[:, :], in0=M[:, :], scalar1=-1.0, scalar2=None, op0=ALU.mult),
              reads=[M_res], writes=[lo_res])
        P.add("dve", lambda e, M=M: e.tensor_scalar(out=steps[:, :], in0=halfpow[:, :], scalar1=M[:, 0:1], scalar2=None, op0=ALU.mult),
              reads=[M_res, "halfpow"], writes=["steps"])
        for k in range(N_IT):
            P.add("dve", lambda e, k=k, lo=lo, thr=thr: e.tensor_tensor(out=thr[:, :], in0=lo[:, :], in1=steps[:, k:k + 1], op=ALU.add),
                  reads=[lo_res, "steps"], writes=[thr_res])
            P.add("dve", lambda e, S=S, thr=thr, cnt=cnt, score=score: e.tensor_scalar(out=cjunk[:, :S], in0=score[:, :S], scalar1=thr[:, 0:1], scalar2=None,
                                                                          op0=ALU.is_ge, op1=ALU.add, accum_out=cnt[:, :]),
                  reads=[score_res, thr_res], writes=["cjunk", cnt_res])
            P.add("dve", lambda e, lb=lb, cnt=cnt, ge=ge: e.tensor_tensor(out=ge[:, :], in0=cnt[:, :], in1=kreq[:, lb:lb + 1], op=ALU.is_ge),
                  reads=[cnt_res, "kreq"], writes=[ge_res])
            P.add("dve", lambda e, k=k, lo=lo, ge=ge: e.scalar_tensor_tensor(out=lo[:, :], in0=ge[:, :], scalar=steps[:, k:k + 1], in1=lo[:, :],
                                                                             op0=ALU.mult, op1=ALU.add),
                  reads=[ge_res, "steps", lo_res], writes=[lo_res])
        P.add("dve", lambda e, S=S, lo=lo, score=score, mneg=mneg: e.tensor_scalar(out=mneg[:, :S], in0=score[:, :S], scalar1=lo[:, 0:1], scalar2=NEG,
                                                           op0=ALU.is_lt, op1=ALU.mult),
              reads=[score_res, lo_res], writes=[mneg_res])
    def attention(lb):
        nkb = 4 * lb + 4
        score, score_res, mneg, mneg_res = ctx[lb]
        P.add("sp", lambda e, lb=lb: e.dma_start(out=QT[:], in_=QT_d[lb * 128:(lb + 1) * 128, :].rearrange("p (h t) -> p h t", t=128)),
              writes=["QT"], dma=True)
        for kb in range(-1, nkb):
            w = NMETA if kb < 0 else 128
            col0 = 0 if kb < 0 else NMETA + 128 * kb
            kt, kt_res = KT_pool.next()
            vt, vt_res = V_pool.next()
            if kb < 0:
                P.add("sp", lambda e, kt=kt: e.dma_start(out=kt[:, :, :NMETA], in_=mKT_d[:, :].rearrange("p (h t) -> p h t", t=NMETA)),
                      writes=[kt_res], dma=True)
                P.add("sp", lambda e, vt=vt: e.dma_start(out=vt[:NMETA, :, 0:128], in_=mV_d[:, :].rearrange("p (h t) -> p h t", t=128)),
                      writes=[vt_res], dma=True)
            else:
                row0 = grow(kb % 4, (kb // 4) * 128, CH["KT"])
                P.add("sp", lambda e, kt=kt, row0=row0: e.dma_start(out=kt[:], in_=g_KT[row0:row0 + 128, :].rearrange("p (h t) -> p h t", t=128)),
                      reads=["g_KT"], writes=[kt_res], dma=True)
                P.add("sp", lambda e, vt=vt, row0=row0: e.dma_start(out=vt[:, :, 0:128], in_=g_V[row0:row0 + 128, :].rearrange("p (h t) -> p h t", t=128)),
                      reads=["g_V"], writes=[vt_res], dma=True)
            for hg in range(2):
                ST, ST_res = ps_ST.next()
                P.add("pe", lambda e, ST=ST, col0=col0, w=w, mneg=mneg: e.matmul(ST[:w, :], lhsT=mneg[:, col0:col0 + w],
                                                                     rhs=ident4[:].rearrange("p a b -> p (a b)"), start=True, stop=False),
                      reads=[mneg_res, "ident4"], writes=[ST_res])
                for h4 in range(4):
                    h = hg * 4 + h4
                    P.add("pe", lambda e, ST=ST, h=h, h4=h4, kt=kt, w=w: e.matmul(ST[:w, h4 * 128:(h4 + 1) * 128], lhsT=kt[:, h, :w],
                                                                                 rhs=QT[:, h, :], start=False, stop=(h4 == 3)),
                          reads=[kt_res, "QT"], writes=[ST_res])
                PT, PT_res = PT_pool.next()
                P.add("act", lambda e, ST=ST, PT=PT, w=w: e.activation(out=PT[:w, :], in_=ST[:w, :], func=AF.Exp, scale=128 ** -0.5),
                      reads=[ST_res], writes=[PT_res])
                for h4 in range(4):
                    h = hg * 4 + h4
                    ob, ob_res, off = obank(h)
                    P.add("pe", lambda e, ob=ob, off=off, PT=PT, h=h, h4=h4, vt=vt, w=w, kb=kb, nkb=nkb: e.matmul(
                        ob[:, off:off + 129], lhsT=PT[:w, h4 * 128:(h4 + 1) * 128], rhs=vt[:w, h, :],
                        start=(kb < 0 and h % 3 == 0), stop=(kb == nkb - 1), skip_group_check=True),
                        reads=[PT_res, vt_res], writes=[ob_res])
        for h in range(8):
            ob, ob_res, off = obank(h)
            rd, rd_res = st1.next()
            P.add("dve", lambda e, ob=ob, off=off, rd=rd: e.reciprocal(out=rd[:, :], in_=ob[:, off + 128:off + 129]),
                  reads=[ob_res], writes=[rd_res])
            P.add("dve", lambda e, ob=ob, off=off, rd=rd, h=h: e.tensor_scalar(out=od[:, h * 128:(h + 1) * 128], in0=ob[:, off:off + 128],
                                                                               scalar1=rd[:, 0:1], scalar2=None, op0=ALU.mult),
                  reads=[ob_res, rd_res], writes=["od"])
        P.add("sp", lambda e, lb=lb: e.dma_start(out=omix_d[lb * 128:(lb + 1) * 128, :], in_=od[:]), reads=["od"], dma=True)
        if stop is not None:
            P.add("sp", lambda e, lb=lb: e.dma_start(out=dbg[lb * 128:(lb + 1) * 128, 0:1024], in_=od[:]), reads=["od"], dma=True)

    if lbs:
        indexer(lbs[0])
    for i, lb in enumerate(lbs):
        if i + 1 < len(lbs):
            indexer(lbs[i + 1])
        bisect(lb)
        attention(lb)
    env.finish()

    env = Env(nc)
    P = env.P
    ones_f, ident_f, ident_bf, mhalf = make_consts(env)
    TRI = env.sb("TRI", [128, 128], F32)
    BLK = env.sb("BLK", [128, 128], F32)
    SEL = env.sb("SEL", [128, 2], F32)
    P.add("pool", lambda e: e.affine_select(out=TRI[:], in_=ones_f[:], pattern=[[1, 128]], compare_op=ALU.is_ge, fill=0.0,
                                            base=0, channel_multiplier=-1), reads=["ones_f"], writes=["TRI"])
    P.add("pool", lambda e: e.memset(TRI[0:64, 64:128], 0.0), writes=["TRI"])
    P.add("pool", lambda e: e.memset(BLK[:], 0.0), writes=["BLK"])
    P.add("pool", lambda e: e.memset(BLK[0:64, 0:64], 1.0), writes=["BLK"])
    P.add("pool", lambda e: e.memset(BLK[64:128, 64:128], 1.0), writes=["BLK"])
    P.add("pool", lambda e: e.memset(SEL[:], 0.0), writes=["SEL"])
    P.add("pool", lambda e: e.memset(SEL[0:64, 0:1], 1.0), writes=["SEL"])
    P.add("pool", lambda e: e.memset(SEL[64:128, 1:2], 1.0), writes=["SEL"])
    glag = env.sb("glag", [128, 256], F32)
    P.add("sp", lambda e: e.dma_start(out=glag[:], in_=gla_g_in[:, :]), writes=["glag"], dma=True)
    Sst = env.sb("Sst", [128, 256], F32)
    P.add("pool", lambda e: e.memset(Sst[:], 0.0), writes=["Sst"])
    Sbf_pool = env.pool("Sbf", 6, [128, 256], BF16)
    q_pool = env.pool("gq", 3, [128, 128], BF16)
    k_pool = env.pool("gk", 3, [128, 128], BF16)
    v_pool = env.pool("gv", 3, [128, 256], BF16)
    lg_pool = env.pool("glg", 3, [128, 128], F32)
    f128 = env.pool("f128", 8, [128, 128], F32)
    b128 = env.pool("b128", 18, [128, 128], BF16)
    qA_pool = env.pool("qA0", 2, [128, 128], BF16)
    qB_pool = env.pool("q0B", 2, [128, 128], BF16)
    dS_pool = env.pool("dSp", 2, [128, 2], F32)
    og_pool = env.pool("ogp", 2, [128, 256], F32)
    junk = env.sb("junkbf", [128, 256], BF16)
    stg = env.pool("gst", 8, [128, 1], F32)
    for (t, r) in qA_pool.bufs + qB_pool.bufs:
        P.add("pool", lambda e, t=t: e.memset(t[:], 0.0), writes=[r])
    ps_bc = env.ps("ps_bc", [128, 512], F32)
    ps_bl = env.ps("ps_bl", [128, 512], F32)
    ps_dS = env.ps("ps_dS", [128, 512], F32)
    ps_att = env.ps("ps_att", [128, 512], F32)
    ps_o = env.ps("ps_o", [128, 512], F32)
    ps_UA = env.ps("ps_UA", [128, 512], F32)
    ps_UB = env.ps("ps_UB", [128, 512], F32)
    ps_tr = env.ps("ps_tr", [128, 2, 128], BF16)
    Sbf, Sbf_res = Sbf_pool.next()
    P.add("pool", lambda e, Sbf=Sbf: e.memset(Sbf[:], 0.0), writes=[Sbf_res])
    cur = {"S": (Sbf, Sbf_res)}
    U_pool = env.pool("Usb", 6, [128, 256], F32)

    hsel = env.sb("hsel", [128, 4], F32)
    P.add("sp", lambda e: e.dma_start(out=hsel[:], in_=hsel_in[:, :]), writes=["hsel"], dma=True)
    qk4_pool = env.pool("qk4", 2, [128, 1024], BF16)
    v4_pool = env.pool("v4", 2, [128, 1024], BF16)
    lg4_pool = env.pool("lg4", 2, [128, 512], F32)
    mqk4 = env.sb("mqk4", [128, 1024], BF16)
    mv4 = env.sb("mv4", [128, 1024], BF16)
    mlg4 = env.sb("mlg4", [128, 512], F32)
    for t, r in ((mqk4, "mqk4"), (mv4, "mv4"), (mlg4, "mlg4")):
        P.add("pool", lambda e, t=t: e.memset(t[:], 0.0), writes=[r])

    def select_head(dst, dst_res, src, src_res, width, col0):
        P.add("dve", lambda e: e.tensor_scalar(out=dst[:], in0=src[:, col0:col0 + width], scalar1=hsel[:, 0:1], scalar2=None, op0=ALU.mult),
              reads=[src_res, "hsel"], writes=[dst_res])
        for hh in range(1, 4):
            P.add("dve", lambda e, hh=hh: e.scalar_tensor_tensor(out=dst[:], in0=src[:, col0 + hh * width:col0 + (hh + 1) * width],
                                                                 scalar=hsel[:, hh:hh + 1], in1=dst[:], op0=ALU.mult, op1=ALU.add),
                  reads=[src_res, "hsel", dst_res], writes=[dst_res])

    if stop == "B2c":
        env.finish()
        return nc, declared
    def og_gather(i):
        R = CH["OG"]
        P.add("pool", lambda e: e.collective_compute(
            "AllGather", ALU.bypass, replica_groups=GROUPS,
            ins=[mine_OG[i * R:(i + 1) * R, :].opt()], outs=[g_OG[i * 4 * R:(i + 1) * 4 * R, :].opt()]),
            reads=[f"og_out_{g}" for g in range(8 * i, 8 * i + 8)], cc=True)

    nblk = DEV["gla_nblk"] if DEV["gla_nblk"] is not None else 64
    def stage1(gb):
        if gb < 0:
            qk4, qk4_res, v4, v4_res, lg4, lg4_res = mqk4, "mqk4", mv4, "mv4", mlg4, "mlg4"
            P.add("sp", lambda e: e.dma_start(out=mqk4[48:64, :], in_=mG_d[:, 0:1024]), writes=["mqk4"], dma=True)
            P.add("sp", lambda e: e.dma_start(out=mv4[48:64, :], in_=mG_d[:, 1024:2048]), writes=["mv4"], dma=True)
            P.add("sp", lambda e: e.dma_start(out=mlg4[48:64, :], in_=mLG_d[:, :]), writes=["mlg4"], dma=True)
        else:
            row0 = grow(gb % 4, (gb // 4) * 128, CH["G"])
            rowl = grow(gb % 4, (gb // 4) * 128, CH["LG"])
            qk4, qk4_res = qk4_pool.next()
            v4, v4_res = v4_pool.next()
            lg4, lg4_res = lg4_pool.next()
            P.add("sp", lambda e, qk4=qk4, row0=row0: e.dma_start(out=qk4[:], in_=g_G[row0:row0 + 128, 0:1024]), reads=["g_G"], writes=[qk4_res], dma=True)
            P.add("sp", lambda e, v4=v4, row0=row0: e.dma_start(out=v4[:], in_=g_G[row0:row0 + 128, 1024:2048]), reads=["g_G"], writes=[v4_res], dma=True)
            P.add("sp", lambda e, lg4=lg4, rowl=rowl: e.dma_start(out=lg4[:], in_=g_LG[rowl:rowl + 128, :]), reads=["g_LG"], writes=[lg4_res], dma=True)
        qt, q_res = q_pool.next()
        kt_, k_res = k_pool.next()
        vt, v_res = v_pool.next()
        lgt, lg_res = lg_pool.next()
        select_head(qt, q_res, qk4, qk4_res, 128, 0)
        select_head(kt_, k_res, qk4, qk4_res, 128, 512)
        select_head(vt, v_res, v4, v4_res, 256, 0)
        select_head(lgt, lg_res, lg4, lg4_res, 128, 0)
        P.add("pe", lambda e, lgt=lgt: e.matmul(ps_bc[:, :128], lhsT=TRI[:], rhs=lgt[:], start=True, stop=True), reads=["TRI", lg_res], writes=["ps_bc"])
        P.add("pe", lambda e, lgt=lgt: e.matmul(ps_bl[:, :128], lhsT=BLK[:], rhs=lgt[:], start=True, stop=True), reads=["BLK", lg_res], writes=["ps_bl"])
        P.add("pe", lambda e, lgt=lgt: e.matmul(ps_dS[:, :128], lhsT=lgt[:], rhs=BLK[:], start=True, stop=True), reads=["BLK", lg_res], writes=["ps_dS"])
        bc, bc_res = f128.next()
        eq, eq_res = f128.next()
        ek, ek_res = f128.next()
        ekd, ekd_res = f128.next()
        dS, dS_res = dS_pool.next()
        P.add("act", lambda e, bc=bc: e.copy(out=bc[:], in_=ps_bc[:, :128]), reads=["ps_bc"], writes=[bc_res])
        P.add("act", lambda e, eq=eq: e.activation(out=eq[:], in_=ps_bc[:, :128], func=AF.Exp), reads=["ps_bc"], writes=[eq_res])
        P.add("act", lambda e, ek=ek: e.activation(out=ek[:], in_=ps_bc[:, :128], func=AF.Exp, scale=-1.0), reads=["ps_bc"], writes=[ek_res])
        P.add("act", lambda e, dS=dS: e.activation(out=dS[:, 0:1], in_=ps_dS[:, 0:1], func=AF.Exp), reads=["ps_dS"], writes=[dS_res])
        P.add("act", lambda e, dS=dS: e.activation(out=dS[:, 1:2], in_=ps_dS[:, 64:65], func=AF.Exp), reads=["ps_dS"], writes=[dS_res])
        P.add("dve", lambda e, bc=bc, ekd=ekd: e.tensor_tensor(out=ekd[:], in0=ps_bl[:, :128], in1=bc[:], op=ALU.subtract),
              reads=["ps_bl", bc_res], writes=[ekd_res])
        P.add("act", lambda e, ekd=ekd: e.activation(out=ekd[:], in_=ekd[:], func=AF.Exp), reads=[ekd_res], writes=[ekd_res])
        qs, qs_res = b128.next()
        ks, ks_res = b128.next()
        kdA, kdA_res = b128.next()
        kdB, kdB_res = b128.next()
        P.add("dve", lambda e, qs=qs, qt=qt, eq=eq: e.tensor_tensor(out=qs[:], in0=qt[:], in1=eq[:], op=ALU.mult), reads=[q_res, eq_res], writes=[qs_res])
        P.add("dve", lambda e, ks=ks, kt_=kt_, ek=ek: e.tensor_tensor(out=ks[:], in0=kt_[:], in1=ek[:], op=ALU.mult), reads=[k_res, ek_res], writes=[ks_res])
        P.add("dve", lambda e, kdA=kdA, kt_=kt_, ekd=ekd: e.scalar_tensor_tensor(out=kdA[:], in0=kt_[:], scalar=SEL[:, 0:1], in1=ekd[:], op0=ALU.mult, op1=ALU.mult),
              reads=[k_res, ekd_res, "SEL"], writes=[kdA_res])
        P.add("dve", lambda e, kdB=kdB, kt_=kt_, ekd=ekd: e.scalar_tensor_tensor(out=kdB[:], in0=kt_[:], scalar=SEL[:, 1:2], in1=ekd[:], op0=ALU.mult, op1=ALU.mult),
              reads=[k_res, ekd_res, "SEL"], writes=[kdB_res])
        P.add("pe", lambda e, qs=qs: e.transpose(out=ps_tr[:, 0, :], in_=qs[:], identity=ident_bf[:]), reads=[qs_res, "ident_bf"], writes=["ps_tr"])
        P.add("pe", lambda e, ks=ks: e.transpose(out=ps_tr[:, 1, :], in_=ks[:], identity=ident_bf[:]), reads=[ks_res, "ident_bf"], writes=["ps_tr"])
        qT, qT_res = b128.next()
        kT, kT_res = b128.next()
        qA, qA_res = qA_pool.next()
        qB, qB_res = qB_pool.next()
        P.add("act", lambda e, qT=qT: e.copy(out=qT[:], in_=ps_tr[:, 0, :]), writes=[qT_res, "ps_tr"])
        P.add("act", lambda e, qA=qA: e.copy(out=qA[:, 0:64], in_=ps_tr[:, 0, 0:64]), writes=[qA_res, "ps_tr"])
        P.add("dve", lambda e, kT=kT: e.tensor_copy(out=kT[:], in_=ps_tr[:, 1, :]), writes=[kT_res, "ps_tr"])
        P.add("dve", lambda e, qB=qB: e.tensor_copy(out=qB[:, 64:128], in_=ps_tr[:, 0, 64:128]), writes=[qB_res, "ps_tr"])
        P.add("pe", lambda e, kT=kT, qT=qT: e.matmul(ps_att[:, :128], lhsT=kT[:], rhs=qT[:], start=True, stop=True), reads=[kT_res, qT_res], writes=["ps_att"])
        am, am_res = b128.next()
        P.add("dve", lambda e, am=am: e.tensor_tensor(out=am[:], in0=ps_att[:, :128], in1=TRI[:], op=ALU.mult), reads=["ps_att", "TRI"], writes=[am_res])
        P.add("pe", lambda e, kdA=kdA, vt=vt: e.matmul(ps_UA[:, :256], lhsT=kdA[:], rhs=vt[:], start=True, stop=True), reads=[kdA_res, v_res], writes=["ps_UA"])
        P.add("pe", lambda e, kdB=kdB, vt=vt: e.matmul(ps_UB[:, :256], lhsT=kdB[:], rhs=vt[:], start=True, stop=True), reads=[kdB_res, v_res], writes=["ps_UB"])
        UA, UA_res = U_pool.next()
        UB, UB_res = U_pool.next()
        P.add("act", lambda e: e.copy(out=UA[:], in_=ps_UA[:, :256]), reads=["ps_UA"], writes=[UA_res])
        P.add("act", lambda e: e.copy(out=UB[:], in_=ps_UB[:, :256]), reads=["ps_UB"], writes=[UB_res])
        return dict(dS=dS, dS_res=dS_res, UA=UA, UA_res=UA_res, UB=UB, UB_res=UB_res, am=am, am_res=am_res, vt=vt, v_res=v_res,
                    qA=qA, qA_res=qA_res, qB=qB, qB_res=qB_res)

    def stage2(gb, d):
        dS, dS_res, UA, UA_res, UB, UB_res = d["dS"], d["dS_res"], d["UA"], d["UA_res"], d["UB"], d["UB_res"]
        am, am_res, vt, v_res, qA, qA_res, qB, qB_res = d["am"], d["am_res"], d["vt"], d["v_res"], d["qA"], d["qA_res"], d["qB"], d["qB_res"]
        SA, SA_res = cur["S"]
        P.add("dve", lambda e: e.scalar_tensor_tensor(out=Sst[:], in0=Sst[:], scalar=dS[:, 0:1], in1=UA[:], op0=ALU.mult, op1=ALU.add),
              reads=["Sst", dS_res, UA_res], writes=["Sst"])
        SB, SB_res = Sbf_pool.next()
        P.add("act", lambda e: e.copy(out=SB[:], in_=Sst[:]), reads=["Sst"], writes=[SB_res])
        P.add("dve", lambda e: e.scalar_tensor_tensor(out=Sst[:], in0=Sst[:], scalar=dS[:, 1:2], in1=UB[:], op0=ALU.mult, op1=ALU.add),
              reads=["Sst", dS_res, UB_res], writes=["Sst"])
        Sn, Sn_res = Sbf_pool.next()
        cur["S"] = (Sn, Sn_res)
        P.add("act", lambda e: e.copy(out=Sn[:], in_=Sst[:]), reads=["Sst"], writes=[Sn_res])
        if gb < 0:
            return
        P.add("pe", lambda e, am=am, vt=vt: e.matmul(ps_o[:, :256], lhsT=am[:], rhs=vt[:], start=True, stop=False), reads=[am_res, v_res], writes=["ps_o"])
        P.add("pe", lambda e, qA=qA, SA=SA: e.matmul(ps_o[:, :256], lhsT=qA[:], rhs=SA[:], start=False, stop=False), reads=[qA_res, SA_res], writes=["ps_o"])
        P.add("pe", lambda e, qB=qB, SB=SB: e.matmul(ps_o[:, :256], lhsT=qB[:], rhs=SB[:], start=False, stop=True), reads=[qB_res, SB_res], writes=["ps_o"])
        ss, ss_res = stg.next()
        ms, ms_res = stg.next()
        rs, rs_res = stg.next()
        P.add("act", lambda e, ss=ss: e.activation(out=junk[:], in_=ps_o[:, :256], func=AF.Square, accum_out=ss[:, :]), reads=["ps_o"], writes=["junk", ss_res])
        P.add("dve", lambda e, ss=ss, ms=ms: e.tensor_scalar(out=ms[:, :], in0=ss[:, :], scalar1=1.0 / 256, scalar2=EPS, op0=ALU.mult, op1=ALU.add),
              reads=[ss_res], writes=[ms_res])
        P.add("pool", lambda e, ms=ms, rs=rs: e.tensor_tensor(out=rs[:, :], in0=ms[:, :], in1=mhalf[:, :], op=ALU.pow), reads=[ms_res, "mhalf"], writes=[rs_res])
        og, og_res = og_pool.next()
        P.add("dve", lambda e, og=og, rs=rs: e.scalar_tensor_tensor(out=og[:], in0=ps_o[:, :256], scalar=rs[:, 0:1], in1=glag[:], op0=ALU.mult, op1=ALU.mult),
              reads=["ps_o", rs_res, "glag"], writes=[og_res])
        P.add("sp", lambda e, og=og, gb=gb: e.dma_start(out=mine_OG[gb * 128:(gb + 1) * 128, :], in_=og[:]), reads=[og_res],
              writes=[f"og_out_{gb}"], dma=True)
        if gb % 8 == 1 and gb >= 9:
            og_gather(gb // 8 - 1)
        if stop is not None:
            P.add("sp", lambda e, og=og, gb=gb: e.dma_start(out=dbg[gb * 128:(gb + 1) * 128, 1024:1280], in_=og[:]), reads=[og_res], dma=True)
    pend_d = stage1(-1)
    for gb in range(-1, nblk):
        nxt = stage1(gb + 1) if gb + 1 < nblk else None
        stage2(gb, pend_d)
        pend_d = nxt
    if nblk == 64:
        og_gather(7)
    env.finish()

    if stop is not None:
        return nc, declared

    env = Env(nc)
    P = env.P
    F = FFNEnv(env, {k: gains[k] for k in ("gmpost", "g2pre", "g2post")})
    xnT, fbuf = F.xnT, F.fbuf
    mixb_pool = F.xnb_pool
    og_view = g_OG.rearrange("(i hh g p) c -> p hh i g c", i=8, hh=4, g=8, p=128)

    def rsel(e):
        if "rsel" not in P.cache:
            P.cache["rsel"] = P.getpid(e) % 4
        return P.cache["rsel"]

    for t in range(4):
        sbs = [128] * 4
        for s in range(4):
            lb = 4 * t + s
            r0 = lb * 128
            od_t, od_res = F.xs_pool.next()
            sg_t, sg_res = F.xs_pool.next()
            P.add("sp", lambda e, od_t=od_t, r0=r0: e.dma_start(out=od_t[:, 0:1024], in_=omix_d[r0:r0 + 128, :]), writes=[od_res], dma=True)
            P.add("pool", lambda e, od_t=od_t, lb=lb: e.dma_start(
                out=od_t[:, 1024:2048].rearrange("p (hh c) -> p hh c", c=256),
                in_=og_view[:, :, lb // 2, 4 * (lb % 2):4 * (lb % 2) + 4, :][:, :, bass.ds(rsel(e), 1), :].rearrange("p hh o c -> p hh (o c)")),
                reads=["g_OG"], writes=[od_res], dma=True)
            P.add("sp", lambda e, sg_t=sg_t, r0=r0: e.dma_start(out=sg_t[:, 0:1024], in_=sg_d[r0:r0 + 128, :]), writes=[sg_res], dma=True)
            mb, mb_res = mixb_pool.next()
            P.add("act", lambda e, mb=mb, od_t=od_t: e.copy(out=mb[:, 0:1024], in_=od_t[:, 0:1024]), reads=[od_res], writes=[mb_res])
            P.add("dve", lambda e, mb=mb, od_t=od_t, sg_t=sg_t: e.tensor_tensor(out=mb[:, 1024:2048], in0=od_t[:, 1024:2048], in1=sg_t[:, 0:1024], op=ALU.mult),
                  reads=[od_res, sg_res], writes=[mb_res])
            F.transpose_into(mb, mb_res, 128, s * 128)
        for cg in range(8):
            wt, wt_res = F.wbig.next()
            P.add("pool", lambda e, wt=wt, cg=cg: e.dma_start(out=wt[:], in_=wts["wout"][:, cg * 256:(cg + 1) * 256].rearrange("(kc p) f -> p kc f", p=128)),
                  writes=[wt_res], dma=True)
            for s in range(4):
                pb, pb_res = F.ps_D.next()
                for kc in range(KC):
                    P.add("pe", lambda e, kc=kc, s=s, pb=pb, wt=wt: e.matmul(pb[:, :256], lhsT=xnT[:, kc, s * 128:(s + 1) * 128], rhs=wt[:, kc, :],
                                                                             start=(kc == 0), stop=(kc == KC - 1)),
                          reads=[wt_res, "xnT"], writes=[pb_res])
                F.evac_copy(fbuf[:, s, cg * 256:(cg + 1) * 256], pb[:, :256], [pb_res], [f"fbuf{s}"])
        for s in range(4):
            r0 = (4 * t + s) * 128
            xs, xs_res = F.xs_pool.next()
            P.add("sp", lambda e, xs=xs, r0=r0: e.dma_start(out=xs[:], in_=hs1_d[r0:r0 + 128, :]), writes=[xs_res], dma=True)
            F.post_norm_residual(fbuf[:, s, :], f"fbuf{s}", 128, "gmpost", 1.0, xs[:], xs_res, fbuf[:, s, :], f"fbuf{s}")
            P.add("sp", lambda e, s=s, r0=r0: e.dma_start(out=hs2_d[r0:r0 + 128, :], in_=fbuf[:, s, :]), reads=[f"fbuf{s}"], writes=[f"hs2_{r0}"], dma=True)
            F.norm_transpose(fbuf[:, s, :], f"fbuf{s}", 128, "g2pre", s * 128)
        F.ffn_core(wts["wg2"], wts["wu2"], wts["wd2"], sbs)
        for s in range(4):
            r0 = (4 * t + s) * 128
            xs, xs_res = F.xs_pool.next()
            P.add("sp", lambda e, xs=xs, r0=r0: e.dma_start(out=xs[:], in_=hs2_d[r0:r0 + 128, :]), reads=[f"hs2_{r0}"], writes=[xs_res], dma=True)
            F.post_norm_residual(fbuf[:, s, :], f"fbuf{s}", 128, "g2post", 0.5, xs[:], xs_res, fbuf[:, s, :], f"fbuf{s}")
            P.add("sp", lambda e, s=s, r0=r0: e.dma_start(out=y[r0:r0 + 128, :], in_=fbuf[:, s, :]), reads=[f"fbuf{s}"], dma=True)
    env.finish()
    return nc, declared


def make_in_maps(inp, declared):
    x = np.asarray(inp["x"], np.float32)
    f = lambda k: np.ascontiguousarray(np.asarray(inp[k], np.float32)[0])
    bc = lambda k, n: np.ascontiguousarray(np.broadcast_to(np.asarray(inp[k], np.float32)[0][None, :], (128, n)))
    names = dict(wg1="ffn1_w_gate", wu1="ffn1_w_up", wd1="ffn1_w_down", win="w_in", wgu="w_gate_up", wout="w_out",
                 wg2="ffn2_w_gate", wu2="ffn2_w_up", wd2="ffn2_w_down")
    gnames = dict(g1pre="ffn1_pre_g", g1post="ffn1_post_g", gmpre="mix_pre_g", gmpost="mix_post_g",
                  g2pre="ffn2_pre_g", g2post="ffn2_post_g")
    shared = {}
    for nm in declared:
        if nm in names:
            shared[nm] = f(names[nm])
        elif nm in gnames:
            shared[nm] = bc(gnames[nm], D)
        elif nm == "bgate":
            shared[nm] = np.asarray(inp["b_gate"], np.float32).reshape(1, 512)
        elif nm == "glag":
            shared[nm] = bc("gla_norm_g", 256)
        elif nm == "meta":
            shared[nm] = np.ascontiguousarray(np.asarray(inp["meta_tokens"], np.float32))
    maps = []
    p = np.arange(128, dtype=np.float32)
    for c in range(NCORES):
        b, r = c // 4, c % 4
        xc = x[b].reshape(64, 128, D)[r::4].reshape(NTOK, D)
        m = dict(shared)
        m["xin"] = np.ascontiguousarray(xc)
        m["qrel"] = (128.0 * r + p).reshape(128, 1).astype(np.float32)
        pos = NMETA + 128.0 * (4 * np.arange(NB)[None, :] + r) + p[:, None]
        m["kreq"] = np.minimum(float(TOPK), pos + 1.0).astype(np.float32)
        hs = np.zeros((128, 4), np.float32)
        hs[:, r] = 1.0
        m["hsel"] = hs
        maps.append(m)
    return maps


_NC_CACHE = {}


def kernel(**inputs):
    if "nc" not in _NC_CACHE:
        _NC_CACHE["nc"] = build_program()
    nc, declared = _NC_CACHE["nc"]
    maps = make_in_maps(inputs, declared)
    res = run_bass_kernel_spmd(nc, maps, core_ids=list(range(NCORES)))
    out = np.zeros((2, 64, 128, D), np.float32)
    for c in range(NCORES):
        b, r = c // 4, c % 4
        out[b, r::4] = np.asarray(res.results[c]["y"], np.float32).reshape(NB, 128, D)
    if DEV["stop"] is not None:
        kernel.dbg = [np.asarray(res.results[c]["dbg"]) for c in range(NCORES)]
    return out.reshape(2, 8192, D)
```
